# Optimizing a Trainium2 kernel written in Bass

```python
import jax, jax.numpy as jnp
from jax import lax
import numpy as np

D_MODEL = 1024
BATCH = 16
SEQ = 2048
DEPTH = 2

CONV_CH = D_MODEL
CONV_WIDTH = 31
HEAD_DIM = 64
N_Q_HEADS = D_MODEL // HEAD_DIM
N_KV_HEADS = N_Q_HEADS // 4
GQA_GROUP = N_Q_HEADS // N_KV_HEADS
ATTN_CH = N_Q_HEADS * HEAD_DIM
KV_CH = N_KV_HEADS * HEAD_DIM
WINDOW = 128
BLOCK = 128
SPAN = BLOCK + 2 * WINDOW
N_GROUPS = 4
EXPERTS_PER_GROUP = 4
TOP_K = 2
D_EXPERT = 512
D_PLE = 256
DN_ALPHA = (2 * DEPTH) ** 0.25
DN_BETA = (8 * DEPTH) ** -0.25
LN_EPS = 1e-5
NEG_INF = -1e30
IN_COLS = 2 * CONV_CH + ATTN_CH + 2 * KV_CH + 2 * D_MODEL
SPLITS = (2 * CONV_CH,
          2 * CONV_CH + ATTN_CH,
          2 * CONV_CH + ATTN_CH + KV_CH,
          2 * CONV_CH + ATTN_CH + 2 * KV_CH,
          2 * CONV_CH + ATTN_CH + 2 * KV_CH + D_MODEL)

kernel_name = 'hybrid_conformer_swa_hmoe_encoder'


def layer_norm(x, g, b):
    xf = x.astype(jnp.float32)
    mu = jnp.mean(xf, axis=-1, keepdims=True)
    var = jnp.mean(jnp.square(xf - mu), axis=-1, keepdims=True)
    y = (xf - mu) * lax.rsqrt(var + LN_EPS)
    return (y * g.astype(jnp.float32) + b.astype(jnp.float32)).astype(x.dtype)


def alibi_slopes(n_heads):
    return 2.0 ** (-8.0 * jnp.arange(1, n_heads + 1, dtype=jnp.float32) / n_heads)


def conformer_conv(u_glu, conv_w, conv_b, ln_g, ln_b):
    a, gate = jnp.split(u_glu, 2, axis=-1)
    u = a * jax.nn.sigmoid(gate)
    pad = CONV_WIDTH // 2
    y = lax.conv_general_dilated(u, conv_w[:, None, :].astype(u.dtype), (1,), [(pad, pad)],
                                 dimension_numbers=('NWC', 'WIO', 'NWC'),
                                 feature_group_count=CONV_CH)
    y = y + conv_b
    y = layer_norm(y, ln_g, ln_b)
    return jax.nn.silu(y)


def windowed_gqa(q, k, v, sink):
    B, S, _ = q.shape
    nb = S // BLOCK
    q = q.reshape(B, S, N_KV_HEADS, GQA_GROUP, HEAD_DIM) * (HEAD_DIM ** -0.5)
    k = k.reshape(B, S, N_KV_HEADS, HEAD_DIM)
    v = v.reshape(B, S, N_KV_HEADS, HEAD_DIM)
    kp = jnp.pad(k, ((0, 0), (WINDOW, WINDOW), (0, 0), (0, 0)))
    vp = jnp.pad(v, ((0, 0), (WINDOW, WINDOW), (0, 0), (0, 0)))
    slopes = alibi_slopes(N_Q_HEADS).reshape(N_KV_HEADS, GQA_GROUP)
    sink = sink.astype(jnp.float32).reshape(N_KV_HEADS, GQA_GROUP)[None, :, :, None]
    rel = jnp.arange(SPAN)[None, :] - WINDOW - jnp.arange(BLOCK)[:, None]
    in_window = jnp.abs(rel) <= WINDOW
    alibi = -slopes[:, :, None, None] * jnp.abs(rel).astype(jnp.float32)[None, None]

    def block(i):
        start = i * BLOCK
        qi = lax.dynamic_slice_in_dim(q, start, BLOCK, axis=1)
        ki = lax.dynamic_slice_in_dim(kp, start, SPAN, axis=1)
        vi = lax.dynamic_slice_in_dim(vp, start, SPAN, axis=1)
        key_pos = start - WINDOW + jnp.arange(SPAN)
        valid = in_window & ((key_pos >= 0) & (key_pos < S))[None, :]
        s = jnp.einsum('bqhgd,bkhd->bhgqk', qi, ki,
                       preferred_element_type=jnp.float32) + alibi
        s = jnp.where(valid, s, NEG_INF)
        m = jnp.maximum(jnp.max(s, axis=-1), sink)
        pr = jnp.exp(s - m[..., None])
        denom = jnp.sum(pr, axis=-1) + jnp.exp(sink - m)
        o = jnp.einsum('bhgqk,bkhd->bqhgd', pr.astype(vi.dtype), vi,
                       preferred_element_type=jnp.float32)
        o = o / jnp.transpose(denom, (0, 3, 1, 2))[..., None]
        return o.astype(q.dtype)

    out = lax.map(block, jnp.arange(nb))
    return jnp.moveaxis(out, 0, 1).reshape(B, S, ATTN_CH)


def hierarchical_moe(x, w_rg, b_rg, w_re, b_re, w1, w3, w2):
    B, S, D = x.shape
    xt = x.reshape(B * S, D)
    xf = xt.astype(jnp.float32)
    g_prob = jax.nn.softmax(xf @ w_rg.astype(jnp.float32) + b_rg.astype(jnp.float32), axis=-1)
    g_w, g_idx = lax.top_k(g_prob, 1)
    e_logits = (xf @ w_re.astype(jnp.float32) + b_re.astype(jnp.float32)).reshape(-1, N_GROUPS, EXPERTS_PER_GROUP)
    e_sel = jnp.take_along_axis(e_logits, g_idx[:, :, None], axis=1)[:, 0]
    e_top, e_idx = lax.top_k(e_sel, TOP_K)
    e_w = jax.nn.softmax(e_top, axis=-1) * g_w
    comb_e = jnp.einsum('tk,tke->te', e_w, jax.nn.one_hot(e_idx, EXPERTS_PER_GROUP, dtype=jnp.float32))
    comb = (jax.nn.one_hot(g_idx[:, 0], N_GROUPS, dtype=jnp.float32)[:, :, None]
            * comb_e[:, None, :]).astype(x.dtype)
    y = jnp.zeros_like(xt)
    for g in range(N_GROUPS):
        h = jax.nn.silu(jnp.einsum('td,edf->tef', xt, w1[g])) * jnp.einsum('td,edf->tef', xt, w3[g])
        y = y + jnp.einsum('tef,efd->td', h * comb[:, g, :, None], w2[g])
    return y.reshape(B, S, D)


def _normal(key, shape, scale):
    return jax.random.normal(key, shape, jnp.float32) * scale


def setup_inputs(seed: int = 0) -> dict:
    key = jax.random.key(seed)
    ks = jax.random.split(key, 32)
    L, D, C, F = DEPTH, D_MODEL, CONV_CH, D_EXPERT
    G, E = N_GROUPS, EXPERTS_PER_GROUP
    return {
        'x': _normal(ks[0], (BATCH, SEQ, D), 1.0),
        'p': _normal(ks[1], (DEPTH, BATCH, SEQ, D_PLE), 1.0),
        'ln_emb_g': 1.0 + _normal(ks[2], (D,), 0.02),
        'ln_emb_b': _normal(ks[3], (D,), 0.02),
        'w_in': _normal(ks[4], (L, D, IN_COLS), D ** -0.5),
        'b_in': _normal(ks[5], (L, IN_COLS), 0.02),
        'conv_w': _normal(ks[6], (L, CONV_WIDTH, C), CONV_WIDTH ** -0.5),
        'conv_b': _normal(ks[7], (L, C), 0.02),
        'conv_ln_g': 1.0 + _normal(ks[8], (L, C), 0.02),
        'conv_ln_b': _normal(ks[9], (L, C), 0.02),
        'w_conv_out': _normal(ks[10], (L, C, D), C ** -0.5 * DN_BETA),
        'w_attn_out': _normal(ks[11], (L, ATTN_CH, D), ATTN_CH ** -0.5 * DN_BETA),
        'attn_sink': _normal(ks[12], (L, N_Q_HEADS), 0.5),
        'w_out': _normal(ks[13], (L, D, D), D ** -0.5 * DN_BETA),
        'ln1_g': 1.0 + _normal(ks[14], (L, D), 0.02),
        'ln1_b': _normal(ks[15], (L, D), 0.02),
        'w_router_group': _normal(ks[16], (L, D, G), D ** -0.5),
        'b_router_group': _normal(ks[17], (L, G), 0.01),
        'w_router_expert': _normal(ks[18], (L, D, G * E), D ** -0.5),
        'b_router_expert': _normal(ks[19], (L, G * E), 0.01),
        'w1': _normal(ks[20], (L, G, E, D, F), D ** -0.5),
        'w3': _normal(ks[21], (L, G, E, D, F), D ** -0.5),
        'w2': _normal(ks[22], (L, G, E, F, D), F ** -0.5 * DN_BETA),
        'w_p': _normal(ks[23], (L, D_PLE, D), D_PLE ** -0.5 * DN_BETA),
        'w_pg': _normal(ks[24], (L, D, D), D ** -0.5),
        'b_pg': _normal(ks[25], (L, D), 0.02),
        'ln2_g': 1.0 + _normal(ks[26], (L, D), 0.02),
        'ln2_b': _normal(ks[27], (L, D), 0.02),
    }


def reference(x, p, ln_emb_g, ln_emb_b, w_in, b_in, conv_w, conv_b, conv_ln_g, conv_ln_b,
              w_conv_out, w_attn_out, attn_sink, w_out, ln1_g, ln1_b,
              w_router_group, b_router_group, w_router_expert, b_router_expert,
              w1, w3, w2, w_p, w_pg, b_pg, ln2_g, ln2_b):
    x = layer_norm(x, ln_emb_g, ln_emb_b)
    for i in range(DEPTH):
        proj = x @ w_in[i] + b_in[i]
        u_glu, q, k, v, g_conv, g_attn = jnp.split(proj, SPLITS, axis=-1)
        y_conv = conformer_conv(u_glu, conv_w[i], conv_b[i], conv_ln_g[i], conv_ln_b[i]) @ w_conv_out[i]
        y_attn = windowed_gqa(q, k, v, attn_sink[i]) @ w_attn_out[i]
        merged = jax.nn.sigmoid(g_conv) * y_conv + jax.nn.sigmoid(g_attn) * y_attn
        x = layer_norm(DN_ALPHA * x + merged @ w_out[i], ln1_g[i], ln1_b[i])
        ffn = hierarchical_moe(x, w_router_group[i], b_router_group[i], w_router_expert[i],
                               b_router_expert[i], w1[i], w3[i], w2[i])
        ple = jax.nn.sigmoid(x @ w_pg[i] + b_pg[i]) * (p[i] @ w_p[i])
        x = layer_norm(DN_ALPHA * x + ffn + ple, ln2_g[i], ln2_b[i])
    return x
```

```python
import numpy as np
from contextlib import ExitStack
import concourse.bass as bass
import concourse.mybir as mybir
from concourse.bass_utils import run_bass_kernel_spmd

F32 = mybir.dt.float32
BF16 = mybir.dt.bfloat16
AF = mybir.ActivationFunctionType
ALU = mybir.AluOpType
AX = mybir.AxisListType

D = 1024
KC = 8
DEPTH = 2
NCORES = 8
IN_COLS = 5632
DN_ALPHA = float((2 * DEPTH) ** 0.25)
LN_EPS = 1e-5
BIG = 1.0e9

N_SP_SLOTS = 16
N_POOL_SLOTS = 24
N_DMA_SLOTS = N_SP_SLOTS + N_POOL_SLOTS


class _Rec:
    def __init__(self):
        self.call = None

    def __getattr__(self, name):
        def f(*a, **k):
            self.call = (name, a, k)
            return None
        return f


def _bind(fn):
    r = _Rec()
    fn(r)
    assert r.call is not None
    return r.call


class Sched:
    ENG = ('pe', 'act', 'dve', 'pool', 'sp')

    def __init__(self, nc):
        self.nc = nc
        self.ops = {e: [] for e in self.ENG}
        self.last_w = {}
        self.readers = {}
        self.dma_count = [0] * N_DMA_SLOTS
        self.rr = {'sp': 0, 'pool': 0, 'act': 0}
        self.known = {e: {} for e in self.ENG}
        self.out_toks = []

    def _deps(self, reads, writes):
        toks = []
        for k in reads:
            t = self.last_w.get(k)
            if t is not None:
                toks.append(t)
        for k in writes:
            t = self.last_w.get(k)
            if t is not None:
                toks.append(t)
            toks.extend(self.readers.get(k, ()))
        return toks

    def _reduce(self, eng, toks):
        best = {}
        for t in toks:
            src = (t[0], t[1])
            if best.get(src, -1) < t[2]:
                best[src] = t[2]
        waits = []
        kn = self.known[eng]
        for src, v in best.items():
            if kn.get(src, -1) >= v:
                continue
            kn[src] = v
            waits.append((src, v))
            if src[0] == 'c':
                self.ops[src[1]][v]['signal'] = True
        return waits

    def _commit(self, tok, reads, writes):
        for k in reads:
            self.readers.setdefault(k, []).append(tok)
        for k in writes:
            self.last_w[k] = tok
            self.readers[k] = []

    def op(self, eng, fn, reads=(), writes=()):
        toks = self._deps(reads, writes)
        if eng == 'pe':
            toks = [t for t in toks if not (t[0] == 'c' and t[1] == 'pe')]
        waits = self._reduce(eng, toks)
        idx = len(self.ops[eng])
        self.ops[eng].append(dict(fn=_bind(fn), waits=waits, signal=False, dma=None))
        tok = ('c', eng, idx)
        self._commit(tok, reads, writes)
        return tok

    def dma(self, eng, fn, reads=(), writes=()):
        toks = self._deps(reads, writes)
        if eng == 'pool':
            slot = N_SP_SLOTS + self.rr['pool']
            self.rr['pool'] = (self.rr['pool'] + 1) % N_POOL_SLOTS
        else:
            slot = self.rr['sp']
            self.rr['sp'] = (self.rr['sp'] + 1) % N_SP_SLOTS
        cnt = self.dma_count[slot]
        if cnt > 0:
            toks.append(('d', slot, cnt))
        self.dma_count[slot] = cnt + 1
        waits = self._reduce(eng, toks)
        self.ops[eng].append(dict(fn=_bind(fn), waits=waits, signal=False, dma=slot))
        tok = ('d', slot, cnt + 1)
        self._commit(tok, reads, writes)
        return tok

    def barrier(self):
        toks = []
        for e in ('pe', 'act', 'dve'):
            idx = len(self.ops[e]) - 1
            while idx >= 0 and self.ops[e][idx]['fn'] is None:
                idx -= 1
            if idx >= 0:
                toks.append(('c', e, idx))
        for s in range(N_SP_SLOTS):
            if self.dma_count[s] > 0:
                toks.append(('d', s, self.dma_count[s]))
        for e in ('pe', 'act', 'dve', 'sp'):
            mine = [t for t in toks if not (t[0] == 'c' and t[1] == e)]
            waits = self._reduce(e, mine)
            if waits:
                self.ops[e].append(dict(fn=None, waits=waits, signal=False, dma=None))

    def finish(self, eng='sp'):
        waits = self._reduce(eng, list(self.out_toks))
        self.ops[eng].append(dict(fn=None, waits=waits, signal=False, dma=None))

    def emit(self):
        nc = self.nc
        with ExitStack() as st:
            csem = {e: st.enter_context(nc.semaphore('c_' + e)) for e in self.ENG}
            dsem = [st.enter_context(nc.semaphore('d%d' % i)) for i in range(N_DMA_SLOTS)]
            sigcnt = {}
            for e in self.ENG:
                c = 0
                arr = []
                for o in self.ops[e]:
                    if o['signal']:
                        c += 1
                    arr.append(c)
                sigcnt[e] = arr
            block = st.enter_context(nc.Block())

            def make(e):
                def body(eng):
                    for o in self.ops[e]:
                        for src, v in o['waits']:
                            if src[0] == 'c':
                                eng.wait_ge(csem[src[1]], sigcnt[src[1]][v])
                            else:
                                eng.wait_ge(dsem[src[1]], 16 * v)
                        if o['fn'] is None:
                            continue
                        nm_, a_, k_ = o['fn']
                        ins = getattr(eng, nm_)(*a_, **k_)
                        if o['dma'] is not None:
                            ins.then_inc(dsem[o['dma']], 16)
                        elif o['signal']:
                            ins.then_inc(csem[e], 1)
                return body

            if self.ops['sp']:
                block.sync(make('sp'))
            if self.ops['pe']:
                block.tensor(make('pe'))
            if self.ops['act']:
                block.scalar(make('act'))
            if self.ops['dve']:
                block.vector(make('dve'))
            if self.ops['pool']:
                block.gpsimd(make('pool'))


def fm_layout(L):
    off = {}
    o = 0
    for l in range(L):
        off[('b_in', l)] = o; o += 44
        off[('conv_w', l)] = o; o += 8 * 31
        off[('conv_b', l)] = o; o += 8
        off[('cln_g', l)] = o; o += 8
        off[('cln_b', l)] = o; o += 8
    return off, o


def bc_layout(L):
    off = {}
    o = 0
    off['emb_g'] = o; o += D
    off['emb_b'] = o; o += D
    for l in range(L):
        for nm in ('ln1_g', 'ln1_b', 'ln2_g', 'ln2_b', 'b_pg'):
            off[(nm, l)] = o; o += D
        off[('b_v', l)] = o; o += 256
        off[('b_r', l)] = o; o += 32
        off[('sink', l)] = o; o += 32
    return off, o


def q_perm():
    perm = np.zeros(1024, dtype=np.int64)
    n = 0
    for p in range(2):
        for g in range(4):
            for e in range(2):
                for d in range(64):
                    perm[n] = ((2 * p + e) * 4 + g) * 64 + d
                    n += 1
    return perm


def slope(kv, g):
    hq = kv * 4 + g
    return float(2.0 ** (-8.0 * (hq + 1) / 16.0))


def build_nc(nseq, S, depth=DEPTH, debug=False):
    NT = S // 128
    NB = S // 512
    L = depth
    nc = bass.Bass("TRN2", target_bir_lowering=False)
    fmo, NV = fm_layout(L)
    bco, NBC = bc_layout(L)

    def din(name, shape, dt=F32):
        return nc.dram_tensor(name, list(shape), dt, kind="ExternalInput").ap()

    x_d = din("x", [nseq, S, D])
    p_d = din("p", [L, nseq, S, 256])
    w_in_d = din("w_in", [L, D, IN_COLS])
    w_co_d = din("w_conv_out", [L, D, D])
    w_ao_d = din("w_attn_out", [L, D, D])
    w_o_d = din("w_out", [L, D, D])
    w_r_d = din("w_r", [L, D, 32])
    w1_d = din("w1", [L, 16, D, 512])
    w3_d = din("w3", [L, 16, D, 512])
    w2_d = din("w2", [L, 16, 512, D])
    w_p_d = din("w_p", [L, 256, D])
    w_pg_d = din("w_pg", [L, D, D])
    vfm_d = din("vfm", [128, NV])
    vbc_d = din("vbc", [NBC])
    alibi_d = din("alibi", [2, 2, 128, 6, 512], BF16)
    ident_d = din("ident", [128, 128])
    out_d = nc.dram_tensor("out", [nseq, S, D], F32, kind="ExternalOutput").ap()
    xs_d = nc.dram_tensor("xres", [nseq, S, D], F32, kind="Internal").ap()
    dbg_d = nc.dram_tensor("dbg", [4, nseq, S, D], F32, kind="ExternalOutput").ap() if debug else None

    WORK_BYTES = 42 * 1024

    with ExitStack() as st:
        def sb(name, shape, dt):
            return st.enter_context(nc.sbuf_tensor(name, list(shape), dt))

        xT = sb("xT", [128, KC, S], BF16)
        r2 = sb("r2", [128, 32768 * S // 2048], BF16)
        ring = [sb("ring%d" % i, [128, 4096], BF16) for i in range(6)]
        work = sb("work", [128, WORK_BYTES // 2], BF16)
        ident_f = sb("ident_f", [128, 128], F32)
        ident_b = sb("ident_b", [128, 128], BF16)
        ones_b = sb("ones_b", [128, 128], BF16)
        vfm = sb("vfm_s", [128, NV], F32)
        bq8 = sb("bq8", [128, L * 8], F32)
        gb = sb("gb", [128, 2, D], F32)
        bv_bc = sb("bv_bc", [128, 256], F32)
        br_bc = sb("br_bc", [128, 32], F32)
        esink = sb("esink", [128, 32], F32)
        bpg_h = sb("bpg_h", [1, D], BF16)
        bpg_l = sb("bpg_l", [1, D], BF16)
        comb = sb("comb", [128, NT, 16], F32)
        Mk = [sb("Mk%d" % i, [128, 128], BF16) for i in range(2)]
        sinkL = sb("sinkL", [64, 128], BF16)
        sinkR = [sb("sinkR%d" % i, [64, 512], BF16) for i in range(2)]
        onesf = sb("onesf", [64, 128], F32)
        ps = [st.enter_context(nc.psum_tensor("ps%d" % i, [128, 512], F32)) for i in range(8)]

        half = 16384 * S // 2048
        bufA = r2[:, 0:half].rearrange("p (c s) -> p c s", c=KC)
        bufM = r2[:, half:2 * half].rearrange("p (c s) -> p c s", c=KC)
        acc = r2[:, :].bitcast(F32).rearrange("p (t d) -> p t d", d=D)

        Sx = Sched(nc)

        wstate = {'off': 0}

        def wreset():
            wstate['off'] = 0

        def walloc(shape, dt):
            n = int(np.prod(shape))
            nbytes = n * (4 if dt == F32 else 2)
            nbytes = (nbytes + 63) // 64 * 64
            o = wstate['off']
            assert o + nbytes <= WORK_BYTES, ("work overflow", o, nbytes)
            wstate['off'] = o + nbytes
            v = work[:, o // 2:(o + nbytes) // 2]
            if dt == F32:
                v = v.bitcast(F32)
            v = v[:, 0:n]
            if len(shape) == 2:
                v = v.rearrange("p (a b) -> p a b", a=shape[0])
            elif len(shape) == 3:
                v = v.rearrange("p (a b c) -> p a b c", a=shape[0], b=shape[1])
            return v

        rstate = {'n': 0}

        def wpanel(src_ap, kc, ncols):
            slot = rstate['n'] % 6
            rstate['n'] += 1
            dst = ring[slot][:, 0:kc * ncols].rearrange("p (k n) -> p k n", k=kc)
            src = src_ap.rearrange("(k p) n -> p k n", p=128)
            key = ('W', slot)
            Sx.dma('pool', lambda e: e.dma_start(out=dst, in_=src), writes=[key])
            return dst, key

        def mm(out, lhsT, rhs, start, stop, reads, writes):
            Sx.op('pe', lambda e: e.matmul(out, lhsT=lhsT, rhs=rhs, start=start, stop=stop),
                  reads=reads, writes=writes)

        def xT_keys(tb):
            return [('xT', tb * 4 + i) for i in range(4)]

        Sx.dma('sp', lambda e: e.dma_start(out=ident_f[:], in_=ident_d[:, :]), writes=['ident_f'])
        Sx.dma('sp', lambda e: e.dma_start(out=vfm[:], in_=vfm_d[:, :]), writes=['vfm'])
        Sx.op('dve', lambda e: e.tensor_copy(out=ident_b[:], in_=ident_f[:]), reads=['ident_f'], writes=['ident_b'])
        Sx.op('dve', lambda e: e.memset(ones_b[:], 1.0), writes=['ones_b'])
        Sx.op('dve', lambda e: e.memset(onesf[:], 1.0), writes=['onesf'])
        Sx.op('dve', lambda e: e.memset(sinkL[:], 0.0), writes=['sinkL'])
        Sx.op('dve', lambda e: e.memset(sinkL[0:1, 0:64], 1.0), writes=['sinkL'])
        Sx.op('dve', lambda e: e.memset(sinkL[32:33, 64:128], 1.0), writes=['sinkL'])
        for i_ in range(2):
            Sx.op('dve', lambda e: e.memset(Mk[i_][:], 0.0), writes=['Mk'])
            Sx.op('dve', lambda e: e.memset(Mk[i_][:, i_ * 64:(i_ + 1) * 64], 1.0), writes=['Mk'])
            Sx.op('dve', lambda e: e.memset(sinkR[i_][:], 0.0), writes=[('sinkR', i_)])
        for l in range(L):
            o = fmo[('b_in', l)] + 16
            Sx.op('dve', lambda e, o=o, l=l: e.tensor_scalar(out=bq8[:, l * 8:(l + 1) * 8], in0=vfm[:, o:o + 8],
                                                              scalar1=0.125, scalar2=None, op0=ALU.mult),
                  reads=['vfm'], writes=[('bq8', l)])

        def load_gb(goff, boff):
            Sx.dma('sp', lambda e: e.dma_start(out=gb[:, 0, :], in_=vbc_d[goff:goff + D].partition_broadcast(128)),
                   writes=['gb0'])
            Sx.dma('sp', lambda e: e.dma_start(out=gb[:, 1, :], in_=vbc_d[boff:boff + D].partition_broadcast(128)),
                   writes=['gb1'])

        NLN = 6

        def ln_phase(ln, pre, prefetch, dst, write_xT, final, nt, pre_pe=None):
            info = {}

            def st0(tt):
                z, zkeys = pre(tt)
                info[tt] = (z, zkeys)
                b = tt % NLN
                stats, mv, rs = ln['stats'][b], ln['mv'][b], ln['rs'][b]
                kb = 'ln%d' % b
                for h in range(2):
                    Sx.op('dve', lambda e: e.bn_stats(out=stats[:, h * 6:(h + 1) * 6], in_=z[:, h * 512:(h + 1) * 512]),
                          reads=zkeys, writes=[(kb, 'st', h)])
                Sx.op('dve', lambda e: e.bn_aggr(out=mv[:, :], in_=stats[:, :]),
                      reads=[(kb, 'st', 0), (kb, 'st', 1)], writes=[(kb, 'mv')])
                Sx.op('dve', lambda e: e.tensor_scalar(out=rs[:, 0:1], in0=mv[:, 1:2], scalar1=LN_EPS, scalar2=None,
                                                       op0=ALU.add),
                      reads=[(kb, 'mv')], writes=[(kb, 'rs0')])

            def st1(tt):
                b = tt % NLN
                rs = ln['rs'][b]
                kb = 'ln%d' % b
                Sx.op('act', lambda e: e.activation(out=rs[:, 0:1], in_=rs[:, 0:1], func=AF.Sqrt),
                      reads=[(kb, 'rs0')], writes=[(kb, 'rs0')])

            def st2(tt):
                b = tt % NLN
                mv, rs = ln['mv'][b], ln['rs'][b]
                kb = 'ln%d' % b
                Sx.op('dve', lambda e: e.reciprocal(out=rs[:, 0:1], in_=rs[:, 0:1]),
                      reads=[(kb, 'rs0')], writes=[(kb, 'rs0')])
                Sx.op('dve', lambda e: e.scalar_tensor_tensor(out=rs[:, 1:2], in0=mv[:, 0:1], scalar=-1.0, in1=rs[:, 0:1],
                                                              op0=ALU.mult, op1=ALU.mult),
                      reads=[(kb, 'mv'), (kb, 'rs0')], writes=[(kb, 'rs1')])

            def st3(tt):
                b = tt % NLN
                rs = ln['rs'][b]
                kb = 'ln%d' % b
                z, zkeys = info[tt]
                Sx.op('act', lambda e: e.activation(out=z, in_=z, func=AF.Identity, bias=rs[:, 1:2], scale=rs[:, 0:1]),
                      reads=zkeys + [(kb, 'rs0'), (kb, 'rs1')], writes=zkeys)

            def st4(tt):
                z, zkeys = info[tt]
                xb = ln['xb'][tt % 4]
                xk = ('lnxb', tt % 4)
                pbank = 4 + tt % 4
                dst_d, dst_key = dst(tt)
                Sx.op('dve', lambda e: e.tensor_tensor(out=z, in0=z, in1=gb[:, 0, :], op=ALU.mult),
                      reads=zkeys + ['gb0'], writes=zkeys)
                Sx.op('dve', lambda e: e.tensor_tensor(out=z, in0=z, in1=gb[:, 1, :], op=ALU.add),
                      reads=zkeys + ['gb1'], writes=zkeys)
                t = Sx.dma('sp', lambda e: e.dma_start(out=dst_d, in_=z), reads=zkeys, writes=[dst_key])
                if final:
                    Sx.out_toks.append(t)
                if write_xT:
                    Sx.op('act', lambda e: e.activation(out=xb[:, :], in_=z, func=AF.Identity),
                          reads=zkeys, writes=[xk])
                    pview = ps[pbank][:, :].bitcast(BF16).rearrange("p (k n) -> p k n", k=KC)
                    for k in range(KC):
                        Sx.op('pe', lambda e: e.transpose(out=pview[:, k, :], in_=xb[:, k * 128:(k + 1) * 128],
                                                          identity=ident_b[:]),
                              reads=[xk, 'ident_b'], writes=[('ps', pbank)])

            def st5(tt):
                if write_xT:
                    pbank = 4 + tt % 4
                    pview = ps[pbank][:, :].bitcast(BF16).rearrange("p (k n) -> p k n", k=KC)
                    Sx.op('act', lambda e: e.activation(out=xT[:, :, tt * 128:(tt + 1) * 128], in_=pview, func=AF.Identity),
                          reads=[('ps', pbank)], writes=[('xT', tt)])

            stages = [st0, st1, st2, st3, st4, st5]
            if prefetch is not None:
                for tt in range(min(2, nt)):
                    prefetch(tt)
            if pre_pe is not None:
                pre_pe(0)
            for step in range(nt + len(stages) - 1):
                if pre_pe is not None and step + 1 < nt:
                    pre_pe(step + 1)
                for k, f in enumerate(stages):
                    tt = step - k
                    if 0 <= tt < nt:
                        f(tt)
                if prefetch is not None and step + 2 < nt:
                    prefetch(step + 2)

        def ln_bufs():
            return dict(stats=[walloc([12], F32) for _ in range(NLN)],
                        mv=[walloc([2], F32) for _ in range(NLN)],
                        rs=[walloc([2], F32) for _ in range(NLN)],
                        xb=[walloc([D], BF16) for _ in range(4)])

        for s in range(nseq):
            Sx.barrier()
            wreset()
            ln = ln_bufs()
            zin = [walloc([D], F32) for _ in range(NLN)]
            load_gb(bco['emb_g'], bco['emb_b'])

            def emb_load(tt):
                b = tt % NLN
                Sx.dma('sp', lambda e: e.dma_start(out=zin[b][:, :], in_=x_d[s, tt * 128:(tt + 1) * 128, :]),
                       writes=[('zin', b)])
            ln_phase(ln, lambda tt: (zin[tt % NLN][:, :], [('zin', tt % NLN)]), emb_load,
                     lambda tt: (xs_d[s, tt * 128:(tt + 1) * 128, :], ('xs', tt)), True, False, NT)
            if debug:
                t_ = Sx.dma('sp', lambda e: e.dma_start(out=dbg_d[0, s], in_=xs_d[s]), reads=[('xs', tt) for tt in range(NT)])
                Sx.out_toks.append(t_)
            for l in range(L):
                if debug and l == 1:
                    t_ = Sx.dma('sp', lambda e: e.dma_start(out=dbg_d[2, s], in_=xs_d[s]), reads=[('xs', tt) for tt in range(NT)])
                    Sx.out_toks.append(t_)
                bo = fmo[('b_in', l)]
                cwo = fmo[('conv_w', l)]
                cbo = fmo[('conv_b', l)]
                cgo = fmo[('cln_g', l)]
                cbb = fmo[('cln_b', l)]
                last_layer = (l == L - 1)

                Sx.barrier()
                wreset()
                ubuf = [walloc([S + 30], BF16) for _ in range(2)]
                diag = [walloc([31, 128], BF16) for _ in range(2)]
                sgb = [walloc([512], F32) for _ in range(2)]
                ysq = [walloc([512], BF16) for _ in range(2)]
                mean_t = [walloc([512], F32) for _ in range(2)]
                rstd_t = [walloc([512], F32) for _ in range(2)]
                t1b = [walloc([512], F32) for _ in range(2)]
                for b in range(2):
                    Sx.op('dve', lambda e, b=b: e.memset(ubuf[b][:, 0:15], 0.0), writes=[('u', b)])
                    Sx.op('dve', lambda e, b=b: e.memset(ubuf[b][:, S + 15:S + 30], 0.0), writes=[('u', b)])
                cstate = {'cnt': 0, 'W': None}

                def glu(c):
                    j, cc = c // 4, c % 4
                    if cc == 0:
                        Wa, ka = wpanel(w_in_d[l, :, j * 512:(j + 1) * 512], KC, 512)
                        Wg, kg = wpanel(w_in_d[l, :, 1024 + j * 512:1024 + (j + 1) * 512], KC, 512)
                        cstate['W'] = (Wa, ka, Wg, kg)
                    Wa, ka, Wg, kg = cstate['W']
                    ub = c % 2
                    for tb in range(NB):
                        cnt = cstate['cnt']
                        pa, pg = (cnt % 2), 2 + (cnt % 2)
                        sg = sgb[cnt % 2]
                        skey = ('sg', cnt % 2)
                        cstate['cnt'] += 1
                        for k in range(KC):
                            mm(ps[pa][:, :], Wa[:, k, cc * 128:(cc + 1) * 128], xT[:, k, tb * 512:(tb + 1) * 512],
                               k == 0, k == KC - 1, [ka] + xT_keys(tb), [('ps', pa)])
                        for k in range(KC):
                            mm(ps[pg][:, :], Wg[:, k, cc * 128:(cc + 1) * 128], xT[:, k, tb * 512:(tb + 1) * 512],
                               k == 0, k == KC - 1, [kg] + xT_keys(tb), [('ps', pg)])
                        Sx.op('act', lambda e: e.activation(
                            out=sg[:, :], in_=ps[pg][:, :], func=AF.Sigmoid, bias=vfm[:, bo + 8 + c:bo + 9 + c]),
                            reads=[('ps', pg), 'vfm'], writes=[skey])
                        Sx.op('dve', lambda e: e.scalar_tensor_tensor(
                            out=ubuf[ub][:, 15 + tb * 512:15 + (tb + 1) * 512], in0=ps[pa][:, :],
                            scalar=vfm[:, bo + c:bo + c + 1], in1=sg[:, :], op0=ALU.add, op1=ALU.mult),
                            reads=[('ps', pa), skey, 'vfm'], writes=[('u', ub)])
                    Sx.op('dve', lambda e: e.tensor_tensor(
                        out=diag[ub][:, :, :],
                        in0=ident_b[:, :].unsqueeze(1).to_broadcast([128, 31, 128]),
                        in1=vfm[:, cwo + c * 31:cwo + (c + 1) * 31].unsqueeze(2).to_broadcast([128, 31, 128]),
                        op=ALU.mult), reads=['ident_b', 'vfm'], writes=[('diag', ub)])

                def conv(c):
                    ub = c % 2
                    for tb in range(NB):
                        pc = 4 + (tb % 2)
                        for k in range(31):
                            mm(ps[pc][:, :], diag[ub][:, k, :], ubuf[ub][:, tb * 512 + k:tb * 512 + k + 512],
                               k == 0, k == 30, [('diag', ub), ('u', ub)], [('ps', pc)])
                        Sx.op('act', lambda e: e.activation(
                            out=bufA[:, c, tb * 512:(tb + 1) * 512], in_=ps[pc][:, :], func=AF.Identity,
                            bias=vfm[:, cbo + c:cbo + c + 1]),
                            reads=[('ps', pc), 'vfm'], writes=[('A', c, tb * 4 + i, e2) for i in range(4) for e2 in range(2)])

                for c in range(KC + 1):
                    if c < KC:
                        glu(c)
                    if c >= 1:
                        conv(c - 1)
                def akeys(c, tb):
                    return [('A', c, tb * 4 + i, e2) for i in range(4) for e2 in range(2)]

                def cstats(tb):
                    mt, rt_ = mean_t[tb % 2], rstd_t[tb % 2]
                    mk_, rk_ = ('mean_t', tb % 2), ('rstd_t', tb % 2)
                    for c in range(KC):
                        yq = ysq[c % 2]
                        Sx.op('act', lambda e: e.activation(
                            out=yq[:, :], in_=bufA[:, c, tb * 512:(tb + 1) * 512], func=AF.Square),
                            reads=akeys(c, tb), writes=[('ysq', c % 2)])
                        mm(ps[6][:, :], ones_b[:, :], bufA[:, c, tb * 512:(tb + 1) * 512], c == 0, c == KC - 1,
                           akeys(c, tb) + ['ones_b'], [('ps', 6)])
                        mm(ps[7][:, :], ones_b[:, :], yq[:, :], c == 0, c == KC - 1,
                           [('ysq', c % 2), 'ones_b'], [('ps', 7)])
                    Sx.op('dve', lambda e: e.tensor_scalar(out=mt[:, :], in0=ps[6][:, :], scalar1=1.0 / D, scalar2=None,
                                                           op0=ALU.mult), reads=[('ps', 6)], writes=[mk_])
                    Sx.op('dve', lambda e: e.tensor_tensor(out=rt_[:, :], in0=mt[:, :], in1=mt[:, :], op=ALU.mult),
                          reads=[mk_], writes=[rk_])
                    Sx.op('dve', lambda e: e.scalar_tensor_tensor(out=rt_[:, :], in0=ps[7][:, :], scalar=1.0 / D,
                                                                  in1=rt_[:, :], op0=ALU.mult, op1=ALU.subtract),
                          reads=[('ps', 7), rk_], writes=[rk_])
                    Sx.op('dve', lambda e: e.tensor_scalar(out=rt_[:, :], in0=rt_[:, :], scalar1=LN_EPS, scalar2=None,
                                                           op0=ALU.add), reads=[rk_], writes=[rk_])
                    Sx.op('act', lambda e: e.activation(out=rt_[:, :], in_=rt_[:, :], func=AF.Sqrt),
                          reads=[rk_], writes=[rk_])
                    Sx.op('dve', lambda e: e.reciprocal(out=rt_[:, :], in_=rt_[:, :]),
                          reads=[rk_], writes=[rk_])

                def cnorm(tb):
                    mt, rt_ = mean_t[tb % 2], rstd_t[tb % 2]
                    mk_, rk_ = ('mean_t', tb % 2), ('rstd_t', tb % 2)
                    for c in range(KC):
                        t1 = t1b[c % 2]
                        Sx.op('dve', lambda e: e.tensor_tensor(
                            out=t1[:, :], in0=bufA[:, c, tb * 512:(tb + 1) * 512], in1=mt[:, :], op=ALU.subtract),
                            reads=akeys(c, tb) + [mk_], writes=[('t1', c % 2)])
                        Sx.op('dve', lambda e: e.tensor_tensor(out=t1[:, :], in0=t1[:, :], in1=rt_[:, :], op=ALU.mult),
                              reads=[('t1', c % 2), rk_], writes=[('t1', c % 2)])
                        Sx.op('act', lambda e: e.activation(
                            out=bufA[:, c, tb * 512:(tb + 1) * 512], in_=t1[:, :], func=AF.Silu,
                            bias=vfm[:, cbb + c:cbb + c + 1], scale=vfm[:, cgo + c:cgo + c + 1]),
                            reads=[('t1', c % 2), 'vfm'], writes=akeys(c, tb))

                for tb in range(NB + 1):
                    if tb < NB:
                        cstats(tb)
                    if tb >= 1:
                        cnorm(tb - 1)

                def gated_branch(w_src, gate_col0, gate_b0, accumulate):
                    cnt2 = 0
                    for j in range(2):
                        Wy, ky = wpanel(w_src[:, j * 512:(j + 1) * 512], KC, 512)
                        Wg2, kg2 = wpanel(w_in_d[l, :, gate_col0 + j * 512:gate_col0 + (j + 1) * 512], KC, 512)
                        for mmi in range(4):
                            m = 4 * j + mmi
                            for tb in range(NB):
                                py, pg2 = (cnt2 % 2), 2 + (cnt2 % 2)
                                sg = sgb[cnt2 % 2]
                                skey = ('sg', cnt2 % 2)
                                tmp = t1b[cnt2 % 2]
                                tkey = ('t1', cnt2 % 2)
                                cnt2 += 1
                                ak = [('A', k2, tb * 4 + i, e2) for k2 in range(KC) for i in range(4) for e2 in range(2)]
                                for k in range(KC):
                                    mm(ps[py][:, :], Wy[:, k, mmi * 128:(mmi + 1) * 128], bufA[:, k, tb * 512:(tb + 1) * 512],
                                       k == 0, k == KC - 1, [ky] + ak, [('ps', py)])
                                for k in range(KC):
                                    mm(ps[pg2][:, :], Wg2[:, k, mmi * 128:(mmi + 1) * 128], xT[:, k, tb * 512:(tb + 1) * 512],
                                       k == 0, k == KC - 1, [kg2] + xT_keys(tb), [('ps', pg2)])
                                Sx.op('act', lambda e, pg2=pg2, sg=sg, m=m: e.activation(
                                    out=sg[:, :], in_=ps[pg2][:, :], func=AF.Sigmoid,
                                    bias=vfm[:, bo + gate_b0 + m:bo + gate_b0 + m + 1]),
                                    reads=[('ps', pg2), 'vfm'], writes=[skey])
                                if not accumulate:
                                    Sx.op('dve', lambda e, py=py, sg=sg, m=m, tb=tb: e.tensor_tensor(
                                        out=bufM[:, m, tb * 512:(tb + 1) * 512], in0=ps[py][:, :], in1=sg[:, :], op=ALU.mult),
                                        reads=[('ps', py), skey], writes=[('M', m, tb)])
                                else:
                                    Sx.op('dve', lambda e, py=py, sg=sg, tmp=tmp: e.tensor_tensor(
                                        out=tmp[:, :], in0=ps[py][:, :], in1=sg[:, :], op=ALU.mult),
                                        reads=[('ps', py), skey], writes=[tkey])
                                    Sx.op('dve', lambda e, tmp=tmp, m=m, tb=tb: e.tensor_tensor(
                                        out=bufM[:, m, tb * 512:(tb + 1) * 512], in0=tmp[:, :],
                                        in1=bufM[:, m, tb * 512:(tb + 1) * 512], op=ALU.add),
                                        reads=[tkey, ('M', m, tb)], writes=[('M', m, tb)])

                gated_branch(w_co_d[l], 3584, 28, False)

                Sx.barrier()
                wreset()
                KTm = walloc([2, S], BF16)
                Vm = walloc([NT, 2, 128], BF16)
                biasH = walloc([6, 512], BF16)
                biasL = walloc([6, 512], BF16)
                PT = [walloc([512], BF16) for _ in range(6)]
                rec = [walloc([512], F32) for _ in range(2)]
                Sx.dma('sp', lambda e: e.dma_start(out=bv_bc[:, :], in_=vbc_d[bco[('b_v', l)]:bco[('b_v', l)] + 256].partition_broadcast(128)),
                       writes=['bv_bc'])
                Sx.dma('sp', lambda e: e.dma_start(out=esink[:, :], in_=vbc_d[bco[('sink', l)]:bco[('sink', l)] + 32].partition_broadcast(128)),
                       writes=['esink'])
                Sx.op('act', lambda e: e.activation(out=esink[:, :], in_=esink[:, :], func=AF.Exp),
                      reads=['esink'], writes=['esink'])
                for p in range(2):
                    for e2 in range(2):
                        for g in range(4):
                            hq = (2 * p + e2) * 4 + g
                            r = 32 * e2
                            Sx.op('dve', lambda e: e.tensor_scalar(
                                out=sinkR[p][r:r + 1, g * 128:(g + 1) * 128], in0=onesf[r:r + 1, 0:128],
                                scalar1=esink[r:r + 1, hq:hq + 1], scalar2=None, op0=ALU.mult),
                                reads=['esink', 'onesf'], writes=[('sinkR', p)])
                Sx.op('dve', lambda e: e.memset(Vm[:, :, :, :], 0.0), writes=[('V', tt) for tt in range(NT)])
                Sx.op('dve', lambda e: e.memset(KTm[:, :, :], 0.0), writes=[('KT', tt) for tt in range(NT)])
                Wkv, kkv = wpanel(w_in_d[l, :, 3072:3584], KC, 512)
                cnt = 0
                for j in range(2):
                    Wq, kq = wpanel(w_in_d[l, :, 2048 + j * 512:2048 + (j + 1) * 512], KC, 512)
                    for cc in range(4):
                        qc = 4 * j + cc
                        for tb in range(NB):
                            pb = 4 + cnt % 2
                            cnt += 1
                            for k in range(KC):
                                mm(ps[pb][:, :], Wq[:, k, cc * 128:(cc + 1) * 128], xT[:, k, tb * 512:(tb + 1) * 512],
                                   k == 0, k == KC - 1, [kq] + xT_keys(tb), [('ps', pb)])
                            Sx.op('act', lambda e: e.activation(
                                out=bufA[:, qc, tb * 512:(tb + 1) * 512], in_=ps[pb][:, :], func=AF.Identity,
                                bias=bq8[:, l * 8 + qc:l * 8 + qc + 1], scale=0.125),
                                reads=[('ps', pb), ('bq8', l)],
                                writes=[('A', qc, tb * 4 + i, e2) for i in range(4) for e2 in range(2)])
                scnt = 0
                ocnt = 0
                pcnt = 0
                for p in range(2):
                    for tb in range(NB):
                        pb = 6 + tb % 2
                        for k in range(KC):
                            mm(ps[pb][:, :], Wkv[:, k, p * 128:(p + 1) * 128], xT[:, k, tb * 512:(tb + 1) * 512],
                               k == 0, k == KC - 1, [kkv] + xT_keys(tb), [('ps', pb)])
                        for ee in range(2):
                            Sx.op('act', lambda e: e.activation(
                                out=KTm[ee * 64:(ee + 1) * 64, ee, tb * 512:(tb + 1) * 512], in_=ps[pb][ee * 64:(ee + 1) * 64, :],
                                func=AF.Identity, bias=vfm[ee * 64:(ee + 1) * 64, bo + 24 + p:bo + 25 + p]),
                                reads=[('ps', pb), 'vfm'], writes=[('KT', tb * 4 + i) for i in range(4)])
                    for tt in range(NT):
                        pb = 6 + tt % 2
                        for k in range(KC):
                            mm(ps[pb][:, 0:128], xT[:, k, tt * 128:(tt + 1) * 128], Wkv[:, k, 256 + p * 128:256 + (p + 1) * 128],
                               k == 0, k == KC - 1, [kkv, ('xT', tt)], [('ps', pb)])
                        for e2 in range(2):
                            Sx.op('dve', lambda e: e.tensor_tensor(
                                out=Vm[:, tt, e2, e2 * 64:(e2 + 1) * 64], in0=ps[pb][:, e2 * 64:(e2 + 1) * 64],
                                in1=bv_bc[:, p * 128 + e2 * 64:p * 128 + (e2 + 1) * 64], op=ALU.add),
                                reads=[('ps', pb), 'bv_bc'], writes=[('V', tt)])
                    Sx.dma('sp', lambda e: e.dma_start(out=biasH[:, :, :], in_=alibi_d[p, 0]), writes=['biasH'])
                    Sx.dma('sp', lambda e: e.dma_start(out=biasL[:, :, :], in_=alibi_d[p, 1]), writes=['biasL'])
                    steps = []
                    for i in range(NT):
                        js = [j for j in (i - 1, i, i + 1) if 0 <= j < NT]
                        nmm = 2 * len(js)
                        c = 0
                        for e2 in range(2):
                            for j in js:
                                steps.append(dict(i=i, e2=e2, j=j, c=c, nmm=nmm, u=ocnt))
                                c += 1
                        ocnt += 1

                    def emit_S(st_):
                        i, e2, j = st_['i'], st_['e2'], st_['j']
                        R0, R1 = e2 * 64, e2 * 64 + 64
                        jrel = j - i + 1
                        pS = st_['t'] % 3
                        Pt = PT[st_['t'] % 6]
                        pk = ('PT', st_['t'] % 6)
                        qkeys = [('A', p * 4 + g, i, ee) for g in range(4) for ee in range(2)]
                        mm(ps[pS][:, :].rearrange("p (g n) -> p g n", g=4), KTm[:, e2, j * 128:(j + 1) * 128],
                           bufA[:, p * 4:(p + 1) * 4, i * 128:(i + 1) * 128],
                           True, False, [('KT', j)] + qkeys, [('ps', pS)])
                        mm(ps[pS][:, :], ident_b[:, :], biasH[:, e2 * 3 + jrel, :], False, False,
                           ['ident_b', 'biasH'], [('ps', pS)])
                        mm(ps[pS][:, :], ident_b[:, :], biasL[:, e2 * 3 + jrel, :], False, True,
                           ['ident_b', 'biasL'], [('ps', pS)])
                        Sx.op('act', lambda e: e.activation(out=Pt[:, :], in_=ps[pS][:, :], func=AF.Exp),
                              reads=[('ps', pS)], writes=[pk])

                    def emit_PV(st_):
                        i, e2, j, c, nmm, u = st_['i'], st_['e2'], st_['j'], st_['c'], st_['nmm'], st_['u']
                        Pt = PT[st_['t'] % 6]
                        pk = ('PT', st_['t'] % 6)
                        pO = 3 + (u % 2)
                        pD = 5 + (u % 2)
                        mm(ps[pO][:, :], Vm[:, j, e2, :], Pt[:, :], c == 0, c == nmm - 1, [('V', j), pk], [('ps', pO)])
                        mm(ps[pD][:, :], Mk[e2][:, :], Pt[:, :], c == 0, False, ['Mk', pk], [('ps', pD)])
                        if c == nmm - 1:
                            rc = rec[u % 2]
                            rk = ('rec', u % 2)
                            qall = [('A', p * 4 + g, i, ee) for g in range(4) for ee in range(2)]
                            mm(ps[pD][:, :], sinkL[0:33, :], sinkR[p][0:33, :], False, True, ['sinkL', ('sinkR', p)], [('ps', pD)])
                            Sx.op('dve', lambda e: e.reciprocal(out=rc[:, :], in_=ps[pD][:, :]), reads=[('ps', pD)], writes=[rk])
                            Sx.op('dve', lambda e: e.tensor_tensor(
                                out=bufA[:, p * 4:(p + 1) * 4, i * 128:(i + 1) * 128],
                                in0=ps[pO][:, :].rearrange("p (g n) -> p g n", g=4),
                                in1=rc[:, :].rearrange("p (g n) -> p g n", g=4), op=ALU.mult),
                                reads=[('ps', pO), rk], writes=qall)

                    LA = 2
                    for t_, st_ in enumerate(steps):
                        st_['t'] = scnt + t_
                    scnt += len(steps)
                    for t_ in range(len(steps) + LA):
                        if t_ < len(steps):
                            emit_S(steps[t_])
                        if t_ - LA >= 0:
                            emit_PV(steps[t_ - LA])

                Sx.barrier()
                wreset()
                sgb = [walloc([512], F32) for _ in range(2)]
                t1b = [walloc([512], F32) for _ in range(2)]
                gated_branch(w_ao_d[l], 4608, 36, True)

                Sx.barrier()
                wreset()
                ln = ln_bufs()
                xr = [walloc([D], F32) for _ in range(NLN)]
                load_gb(bco[('ln1_g', l)], bco[('ln1_b', l)])
                Wo = []
                for j in range(2):
                    Wo.append(wpanel(w_o_d[l, :, j * 512:(j + 1) * 512], KC, 512))

                def xr_load(tt):
                    b = tt % NLN
                    Sx.dma('sp', lambda e: e.dma_start(out=xr[b][:, :], in_=xs_d[s, tt * 128:(tt + 1) * 128, :]),
                           reads=[('xs', tt)], writes=[('xr', b)])

                def c_pe(tt):
                    mkeys = [('M', m, tt // 4) for m in range(KC)]
                    for j in range(2):
                        pb = 2 * (tt % 2) + j
                        for k in range(KC):
                            mm(ps[pb][:, :], bufM[:, k, tt * 128:(tt + 1) * 128], Wo[j][0][:, k, :],
                               k == 0, k == KC - 1, [Wo[j][1]] + mkeys, [('ps', pb)])

                def c_pre(tt):
                    b = tt % NLN
                    for j in range(2):
                        pb = 2 * (tt % 2) + j
                        Sx.op('dve', lambda e: e.scalar_tensor_tensor(
                            out=xr[b][:, j * 512:(j + 1) * 512], in0=xr[b][:, j * 512:(j + 1) * 512], scalar=DN_ALPHA,
                            in1=ps[pb][:, :], op0=ALU.mult, op1=ALU.add),
                            reads=[('ps', pb), ('xr', b)], writes=[('xr', b)])
                    return xr[b][:, :], [('xr', b)]

                ln_phase(ln, c_pre, xr_load, lambda tt: (xs_d[s, tt * 128:(tt + 1) * 128, :], ('xs', tt)), True, False, NT, pre_pe=c_pe)

                if debug:
                    t_ = Sx.dma('sp', lambda e, l=l: e.dma_start(out=dbg_d[1 + 2 * l, s], in_=xs_d[s]), reads=[('xs', tt) for tt in range(NT)])
                    Sx.out_toks.append(t_)
                Sx.barrier()
                wreset()
                lgall = walloc([NT, 32], F32)
                rA = walloc([NT, 16], F32)
                rB = walloc([NT, 16], F32)
                rC = walloc([NT, 16], F32)
                rS = walloc([12, NT], F32)
                rG = walloc([3, NT, 4], F32)
                pin = [walloc([256], F32) for _ in range(4)]
                pTall = walloc([2, S], BF16)
                sgp = [walloc([512], F32) for _ in range(2)]
                tpb = [walloc([512], F32) for _ in range(2)]
                hT = [walloc([4, 512], BF16) for _ in range(2)]
                s1b = [walloc([512], F32) for _ in range(2)]

                def p_load(tt):
                    Sx.dma('sp', lambda e: e.dma_start(out=pin[tt % 4][:, :], in_=p_d[l, s, tt * 128:(tt + 1) * 128, :]),
                           writes=[('pin', tt % 4)])
                for tt in range(min(4, NT)):
                    p_load(tt)
                for tt in range(NT):
                    Sx.dma('sp', lambda e: e.dma_start(out=acc[:, tt, :], in_=xs_d[s, tt * 128:(tt + 1) * 128, :]),
                           reads=[('xs', tt)], writes=[('acc', tt, 0), ('acc', tt, 1)])
                Sx.dma('sp', lambda e: e.dma_start(out=br_bc[:, :], in_=vbc_d[bco[('b_r', l)]:bco[('b_r', l)] + 32].partition_broadcast(128)),
                       writes=['br_bc'])
                bpg_f = gb[0:1, 0, :]
                bpg_t = gb[0:1, 1, :]
                Sx.dma('sp', lambda e: e.dma_start(out=bpg_f, in_=vbc_d[bco[('b_pg', l)]:bco[('b_pg', l)] + D].partition_broadcast(1)),
                       writes=['gb0'])
                Sx.op('dve', lambda e: e.tensor_copy(out=bpg_h[:, :], in_=bpg_f), reads=['gb0'], writes=['bpg_h'])
                Sx.op('dve', lambda e: e.tensor_copy(out=bpg_t, in_=bpg_h[:, :]), reads=['bpg_h'], writes=['gb1'])
                Sx.op('dve', lambda e: e.tensor_tensor(out=bpg_t, in0=bpg_f, in1=bpg_t, op=ALU.subtract),
                      reads=['gb0', 'gb1'], writes=['gb1'])
                Sx.op('dve', lambda e: e.tensor_copy(out=bpg_l[:, :], in_=bpg_t), reads=['gb1'], writes=['bpg_l'])
                Wr, kr = wpanel(w_r_d[l], KC, 32)
                for tt in range(NT):
                    for k in range(KC):
                        mm(ps[0][:, tt * 32:(tt + 1) * 32], xT[:, k, tt * 128:(tt + 1) * 128], Wr[:, k, :], k == 0, k == KC - 1,
                           [kr, ('xT', tt)], [('ps', 0)])
                for tt in range(NT):
                    pb = 6 + tt % 2
                    for k in range(2):
                        Sx.op('pe', lambda e: e.transpose(out=ps[pb][:, k * 128:(k + 1) * 128], in_=pin[tt % 4][:, k * 128:(k + 1) * 128],
                                                          identity=ident_f[:]),
                              reads=[('pin', tt % 4), 'ident_f'], writes=[('ps', pb)])
                    Sx.op('act', lambda e: e.activation(out=pTall[:, :, tt * 128:(tt + 1) * 128],
                                                        in_=ps[pb][:, 0:256].rearrange("p (k n) -> p k n", k=2), func=AF.Identity),
                          reads=[('ps', pb)], writes=[('pT', tt)])
                    if tt + 4 < NT:
                        p_load(tt + 4)
                V = Sx.op
                RK = ['rt']

                def bc(ap2, n):
                    return ap2.unsqueeze(2).to_broadcast([128, NT, n])
                gsh, gmk, pen = rG[:, 0, :, :], rG[:, 1, :, :], rG[:, 2, :, :]
                sc = lambda i: rS[:, i, :]
                em4 = rA[:, :, :].rearrange("p t (g k) -> p t g k", g=4)
                V('dve', lambda e: e.tensor_tensor(out=lgall[:, :, :], in0=ps[0][:, 0:NT * 32].rearrange("p (t n) -> p t n", n=32),
                                                   in1=br_bc[:, :].unsqueeze(1).to_broadcast([128, NT, 32]), op=ALU.add),
                  reads=[('ps', 0), 'br_bc'], writes=RK)
                V('dve', lambda e: e.tensor_reduce(out=sc(0), in_=lgall[:, :, 0:4], axis=AX.X, op=ALU.max), reads=RK, writes=RK)
                V('dve', lambda e: e.tensor_tensor(out=gsh, in0=lgall[:, :, 0:4], in1=bc(sc(0), 4), op=ALU.subtract), reads=RK, writes=RK)
                V('dve', lambda e: e.tensor_scalar(out=gmk, in0=gsh, scalar1=0.0, scalar2=None, op0=ALU.is_ge), reads=RK, writes=RK)
                V('dve', lambda e: e.tensor_scalar(out=pen, in0=gmk, scalar1=-1.0, scalar2=BIG, op0=ALU.add, op1=ALU.mult), reads=RK, writes=RK)
                V('act', lambda e: e.activation(out=gsh, in_=gsh, func=AF.Exp), reads=RK, writes=RK)
                V('dve', lambda e: e.tensor_reduce(out=sc(1), in_=gsh, axis=AX.X, op=ALU.add), reads=RK, writes=RK)
                V('dve', lambda e: e.reciprocal(out=sc(2), in_=sc(1)), reads=RK, writes=RK)
                V('dve', lambda e: e.tensor_tensor(out=em4, in0=lgall[:, :, 4:20].rearrange("p t (g k) -> p t g k", g=4),
                                                   in1=pen.unsqueeze(3).to_broadcast([128, NT, 4, 4]), op=ALU.add), reads=RK, writes=RK)
                V('dve', lambda e: e.tensor_reduce(out=sc(3), in_=rA[:, :, :], axis=AX.X, op=ALU.max), reads=RK, writes=RK)
                V('dve', lambda e: e.tensor_tensor(out=rB[:, :, :], in0=rA[:, :, :], in1=bc(sc(3), 16), op=ALU.is_ge), reads=RK, writes=RK)
                V('dve', lambda e: e.scalar_tensor_tensor(out=rA[:, :, :], in0=rB[:, :, :], scalar=-BIG, in1=rA[:, :, :], op0=ALU.mult, op1=ALU.add),
                  reads=RK, writes=RK)
                V('dve', lambda e: e.tensor_reduce(out=sc(4), in_=rA[:, :, :], axis=AX.X, op=ALU.max), reads=RK, writes=RK)
                V('dve', lambda e: e.tensor_tensor(out=rC[:, :, :], in0=rA[:, :, :], in1=bc(sc(4), 16), op=ALU.is_ge), reads=RK, writes=RK)
                V('dve', lambda e: e.tensor_tensor(out=sc(5), in0=sc(4), in1=sc(3), op=ALU.subtract), reads=RK, writes=RK)
                V('act', lambda e: e.activation(out=sc(6), in_=sc(5), func=AF.Exp), reads=RK, writes=RK)
                V('dve', lambda e: e.tensor_scalar(out=sc(7), in0=sc(6), scalar1=1.0, scalar2=None, op0=ALU.add), reads=RK, writes=RK)
                V('dve', lambda e: e.reciprocal(out=sc(7), in_=sc(7)), reads=RK, writes=RK)
                V('dve', lambda e: e.tensor_tensor(out=sc(8), in0=sc(7), in1=sc(2), op=ALU.mult), reads=RK, writes=RK)
                V('dve', lambda e: e.tensor_tensor(out=sc(9), in0=sc(8), in1=sc(6), op=ALU.mult), reads=RK, writes=RK)
                V('dve', lambda e: e.tensor_tensor(out=rB[:, :, :], in0=rB[:, :, :], in1=bc(sc(8), 16), op=ALU.mult), reads=RK, writes=RK)
                V('dve', lambda e: e.tensor_tensor(out=rC[:, :, :], in0=rC[:, :, :], in1=bc(sc(9), 16), op=ALU.mult), reads=RK, writes=RK)
                V('dve', lambda e: e.tensor_tensor(out=comb[:, :, :], in0=rB[:, :, :], in1=rC[:, :, :], op=ALU.add),
                  reads=RK, writes=[('comb', tt) for tt in range(NT)])
                Wp, kp = wpanel(w_p_d[l], 2, D)
                pcnt2 = 0
                for hf in range(2):
                    Wpg, kpg = wpanel(w_pg_d[l, :, hf * 512:(hf + 1) * 512], KC, 512)
                    for tt in range(NT):
                        b = pcnt2 % 2
                        pw = 1 + pcnt2 % 2
                        pg3 = 3 + pcnt2 % 3
                        pcnt2 += 1
                        for k in range(2):
                            mm(ps[pw][:, :], pTall[:, k, tt * 128:(tt + 1) * 128], Wp[:, k, hf * 512:(hf + 1) * 512], k == 0, k == 1,
                               [kp, ('pT', tt)], [('ps', pw)])
                        for k in range(KC):
                            mm(ps[pg3][:, :], xT[:, k, tt * 128:(tt + 1) * 128], Wpg[:, k, :], k == 0, False,
                               [kpg, ('xT', tt)], [('ps', pg3)])
                        mm(ps[pg3][:, :], ones_b[0:1, :], bpg_h[0:1, hf * 512:(hf + 1) * 512], False, False,
                           ['ones_b', 'bpg_h'], [('ps', pg3)])
                        mm(ps[pg3][:, :], ones_b[0:1, :], bpg_l[0:1, hf * 512:(hf + 1) * 512], False, True,
                           ['ones_b', 'bpg_l'], [('ps', pg3)])
                        Sx.op('act', lambda e: e.activation(out=sgp[b][:, :], in_=ps[pg3][:, :], func=AF.Sigmoid),
                              reads=[('ps', pg3)], writes=[('sgp', b)])
                        Sx.op('dve', lambda e: e.tensor_tensor(out=tpb[b][:, :], in0=ps[pw][:, :], in1=sgp[b][:, :], op=ALU.mult),
                              reads=[('ps', pw), ('sgp', b)], writes=[('tpb', b)])
                        Sx.op('dve', lambda e: e.scalar_tensor_tensor(
                            out=acc[:, tt, hf * 512:(hf + 1) * 512], in0=acc[:, tt, hf * 512:(hf + 1) * 512], scalar=DN_ALPHA,
                            in1=tpb[b][:, :], op0=ALU.mult, op1=ALU.add),
                            reads=[('tpb', b), ('acc', tt, hf)], writes=[('acc', tt, hf)])
                mstate = {'hcnt': 0, 'ycnt': 0, 'W': None}

                def moe_h(u):
                    ex, tb = u // NB, u % NB
                    if tb == 0:
                        W1, k1 = wpanel(w1_d[l, ex], KC, 512)
                        W3, k3 = wpanel(w3_d[l, ex], KC, 512)
                        W2, k2 = wpanel(w2_d[l, ex], 4, D)
                        mstate[('W', ex)] = (W1, k1, W3, k3, W2, k2)
                    W1, k1, W3, k3, W2, k2 = mstate[('W', ex)]
                    hb = u % 2
                    for fc in range(4):
                        hcnt = mstate['hcnt']
                        p1, p3 = (hcnt % 2), 2 + (hcnt % 2)
                        s1 = s1b[hcnt % 2]
                        s1k = ('s1', hcnt % 2)
                        mstate['hcnt'] += 1
                        for k in range(KC):
                            mm(ps[p1][:, :], W1[:, k, fc * 128:(fc + 1) * 128], xT[:, k, tb * 512:(tb + 1) * 512],
                               k == 0, k == KC - 1, [k1] + xT_keys(tb), [('ps', p1)])
                        for k in range(KC):
                            mm(ps[p3][:, :], W3[:, k, fc * 128:(fc + 1) * 128], xT[:, k, tb * 512:(tb + 1) * 512],
                               k == 0, k == KC - 1, [k3] + xT_keys(tb), [('ps', p3)])
                        Sx.op('act', lambda e: e.activation(out=s1[:, :], in_=ps[p1][:, :], func=AF.Silu),
                              reads=[('ps', p1)], writes=[s1k])
                        Sx.op('dve', lambda e: e.tensor_tensor(
                            out=hT[hb][:, fc, :], in0=ps[p3][:, :], in1=s1[:, :], op=ALU.mult),
                            reads=[('ps', p3), s1k], writes=[('hT', hb, fc)])

                def moe_y(u):
                    ex, tb = u // NB, u % NB
                    W1, k1, W3, k3, W2, k2 = mstate[('W', ex)]
                    hb = u % 2
                    for ts in range(4):
                        tt = tb * 4 + ts
                        for hf in range(2):
                            py = 4 + (mstate['ycnt'] % 4)
                            mstate['ycnt'] += 1
                            for fc in range(4):
                                mm(ps[py][:, :], hT[hb][:, fc, ts * 128:(ts + 1) * 128], W2[:, fc, hf * 512:(hf + 1) * 512],
                                   fc == 0, fc == 3, [k2, ('hT', hb, fc)], [('ps', py)])
                            Sx.op('dve', lambda e: e.scalar_tensor_tensor(
                                out=acc[:, tt, hf * 512:(hf + 1) * 512], in0=ps[py][:, :], scalar=comb[:, tt, ex:ex + 1],
                                in1=acc[:, tt, hf * 512:(hf + 1) * 512], op0=ALU.mult, op1=ALU.add),
                                reads=[('ps', py), ('comb', tt), ('acc', tt, hf)], writes=[('acc', tt, hf)])

                NU = 16 * NB
                for u in range(NU + 1):
                    if u < NU:
                        moe_h(u)
                    if u >= 1:
                        moe_y(u - 1)
                Sx.barrier()
                wreset()
                ln = ln_bufs()
                load_gb(bco[('ln2_g', l)], bco[('ln2_b', l)])
                final = last_layer
                ln_phase(ln, lambda tt: (acc[:, tt, :], [('acc', tt, 0), ('acc', tt, 1)]), None,
                         lambda tt: ((out_d if final else xs_d)[s, tt * 128:(tt + 1) * 128, :],
                                     ('out', s, tt) if final else ('xs', tt)),
                         not final, final, NT)
        Sx.finish('sp')
        Sx.emit()
    return nc


def prep_shared(inp, L=DEPTH):
    perm = q_perm()
    w_in = np.array(inp['w_in'], dtype=np.float32, copy=True)
    b_in = np.array(inp['b_in'], dtype=np.float32, copy=True)
    w_in[:, :, 2048:3072] = w_in[:, :, 2048:3072][:, :, perm]
    b_in[:, 2048:3072] = b_in[:, 2048:3072][:, perm]
    w_ao = np.ascontiguousarray(np.asarray(inp['w_attn_out'], dtype=np.float32)[:, perm, :])
    w_r = np.zeros((L, D, 32), dtype=np.float32)
    w_r[:, :, 0:4] = inp['w_router_group']
    w_r[:, :, 4:20] = inp['w_router_expert']
    fmo, NV = fm_layout(L)
    bco, NBC = bc_layout(L)
    vfm = np.zeros((128, NV), dtype=np.float32)
    vbc = np.zeros((NBC,), dtype=np.float32)

    def fm(v):
        return np.asarray(v, dtype=np.float32).reshape(-1, 128).T

    for l in range(L):
        vfm[:, fmo[('b_in', l)]:fmo[('b_in', l)] + 44] = fm(b_in[l])
        cw = np.asarray(inp['conv_w'][l], dtype=np.float32).T.reshape(8, 128, 31).transpose(1, 0, 2).reshape(128, 248)
        vfm[:, fmo[('conv_w', l)]:fmo[('conv_w', l)] + 248] = cw
        vfm[:, fmo[('conv_b', l)]:fmo[('conv_b', l)] + 8] = fm(inp['conv_b'][l])
        vfm[:, fmo[('cln_g', l)]:fmo[('cln_g', l)] + 8] = fm(inp['conv_ln_g'][l])
        vfm[:, fmo[('cln_b', l)]:fmo[('cln_b', l)] + 8] = fm(inp['conv_ln_b'][l])
        for nm, src in (('ln1_g', 'ln1_g'), ('ln1_b', 'ln1_b'), ('ln2_g', 'ln2_g'), ('ln2_b', 'ln2_b'), ('b_pg', 'b_pg')):
            vbc[bco[(nm, l)]:bco[(nm, l)] + D] = inp[src][l]
        vbc[bco[('b_v', l)]:bco[('b_v', l)] + 256] = b_in[l, 3328:3584]
        vbc[bco[('b_r', l)]:bco[('b_r', l)] + 4] = inp['b_router_group'][l]
        vbc[bco[('b_r', l)] + 4:bco[('b_r', l)] + 20] = inp['b_router_expert'][l]
        vbc[bco[('sink', l)]:bco[('sink', l)] + 16] = inp['attn_sink'][l]
    vbc[bco['emb_g']:bco['emb_g'] + D] = inp['ln_emb_g']
    vbc[bco['emb_b']:bco['emb_b'] + D] = inp['ln_emb_b']
    kk = np.arange(128)[:, None].astype(np.float64)
    qq = np.arange(128)[None, :].astype(np.float64)
    Dm = np.zeros((3, 128, 128))
    Dm[0] = np.where(kk >= qq, qq - kk + 128, np.inf)
    Dm[1] = np.abs(kk - qq)
    Dm[2] = np.where(kk <= qq, kk - qq + 128, np.inf)
    import ml_dtypes
    alibi = np.zeros((2, 2, 128, 6, 4, 128), dtype=np.float32)
    for p in range(2):
        for e in range(2):
            for g in range(4):
                sl = slope(2 * p + e, g)
                for j in range(3):
                    a = -sl * Dm[j]
                    a[~np.isfinite(a)] = -1.0e4
                    a = a.astype(np.float32)
                    hi = a.astype(ml_dtypes.bfloat16).astype(np.float32)
                    lo = (a - hi).astype(ml_dtypes.bfloat16).astype(np.float32)
                    alibi[p, 0, :, e * 3 + j, g, :] = hi
                    alibi[p, 1, :, e * 3 + j, g, :] = lo
    shared = dict(
        w_in=w_in, w_conv_out=np.asarray(inp['w_conv_out'], dtype=np.float32), w_attn_out=w_ao,
        w_out=np.asarray(inp['w_out'], dtype=np.float32), w_r=w_r,
        w1=np.asarray(inp['w1'], dtype=np.float32).reshape(L, 16, D, 512),
        w3=np.asarray(inp['w3'], dtype=np.float32).reshape(L, 16, D, 512),
        w2=np.asarray(inp['w2'], dtype=np.float32).reshape(L, 16, 512, D),
        w_p=np.asarray(inp['w_p'], dtype=np.float32), w_pg=np.asarray(inp['w_pg'], dtype=np.float32),
        vfm=vfm, vbc=vbc, alibi=alibi.reshape(2, 2, 128, 6, 512).astype(ml_dtypes.bfloat16), ident=np.eye(128, dtype=np.float32))
    return shared


_NC_CACHE = {}


def run(inp, ncores, nseq, S, depth=DEPTH, trace=False, debug=False):
    key = (nseq, S, depth, debug)
    if key not in _NC_CACHE:
        _NC_CACHE[key] = build_nc(nseq, S, depth, debug)
    nc = _NC_CACHE[key]
    shared = prep_shared(inp, depth)
    x = np.asarray(inp['x'], dtype=np.float32)
    p = np.asarray(inp['p'], dtype=np.float32)
    in_maps = []
    for c in range(ncores):
        m = dict(shared)
        m['x'] = np.ascontiguousarray(x[c * nseq:(c + 1) * nseq])
        m['p'] = np.ascontiguousarray(p[:, c * nseq:(c + 1) * nseq])
        in_maps.append(m)
    res = run_bass_kernel_spmd(nc, in_maps, core_ids=list(range(ncores)), trace=trace)
    out = np.concatenate([r['out'] for r in res.results], axis=0)
    if debug:
        res.dbg = np.concatenate([r['dbg'] for r in res.results], axis=1)
    return out, res


def kernel(**inputs):
    out, _ = run(inputs, NCORES, 2, 2048, DEPTH)
    return out.astype(np.float32)
```

```python
import numpy as np
from contextlib import ExitStack
import concourse.bass as bass
import concourse.mybir as mybir
from concourse.bass_utils import run_bass_kernel_spmd

F32 = mybir.dt.float32
BF16 = mybir.dt.bfloat16
AF = mybir.ActivationFunctionType
ALU = mybir.AluOpType
AX = mybir.AxisListType

D = 1024
KC = 8
DEPTH = 2
NCORES = 8
IN_COLS = 5632
DN_ALPHA = float((2 * DEPTH) ** 0.25)
LN_EPS = 1e-5
BIG = 1.0e9

N_SP_SLOTS = 16
N_POOL_SLOTS = 24
N_DMA_SLOTS = N_SP_SLOTS + N_POOL_SLOTS


class _Rec:
    def __init__(self):
        self.call = None

    def __getattr__(self, name):
        def f(*a, **k):
            self.call = (name, a, k)
            return None
        return f


def _bind(fn):
    r = _Rec()
    fn(r)
    assert r.call is not None
    return r.call


class Sched:
    ENG = ('pe', 'act', 'dve', 'pool', 'sp')

    def __init__(self, nc):
        self.nc = nc
        self.ops = {e: [] for e in self.ENG}
        self.last_w = {}
        self.readers = {}
        self.dma_count = [0] * N_DMA_SLOTS
        self.rr = {'sp': 0, 'pool': 0, 'act': 0}
        self.known = {e: {} for e in self.ENG}
        self.out_toks = []

    def _deps(self, reads, writes):
        toks = []
        for k in reads:
            t = self.last_w.get(k)
            if t is not None:
                toks.append(t)
        for k in writes:
            t = self.last_w.get(k)
            if t is not None:
                toks.append(t)
            toks.extend(self.readers.get(k, ()))
        return toks

    def _reduce(self, eng, toks):
        best = {}
        for t in toks:
            src = (t[0], t[1])
            if best.get(src, -1) < t[2]:
                best[src] = t[2]
        waits = []
        kn = self.known[eng]
        for src, v in best.items():
            if kn.get(src, -1) >= v:
                continue
            kn[src] = v
            waits.append((src, v))
            if src[0] == 'c':
                self.ops[src[1]][v]['signal'] = True
        return waits

    def _commit(self, tok, reads, writes):
        for k in reads:
            self.readers.setdefault(k, []).append(tok)
        for k in writes:
            self.last_w[k] = tok
            self.readers[k] = []

    def op(self, eng, fn, reads=(), writes=()):
        toks = self._deps(reads, writes)
        if eng == 'pe':
            toks = [t for t in toks if not (t[0] == 'c' and t[1] == 'pe')]
        waits = self._reduce(eng, toks)
        idx = len(self.ops[eng])
        self.ops[eng].append(dict(fn=_bind(fn), waits=waits, signal=False, dma=None))
        tok = ('c', eng, idx)
        self._commit(tok, reads, writes)
        return tok

    def dma(self, eng, fn, reads=(), writes=()):
        toks = self._deps(reads, writes)
        if eng == 'pool':
            slot = N_SP_SLOTS + self.rr['pool']
            self.rr['pool'] = (self.rr['pool'] + 1) % N_POOL_SLOTS
        else:
            slot = self.rr['sp']
            self.rr['sp'] = (self.rr['sp'] + 1) % N_SP_SLOTS
        cnt = self.dma_count[slot]
        if cnt > 0:
            toks.append(('d', slot, cnt))
        self.dma_count[slot] = cnt + 1
        waits = self._reduce(eng, toks)
        self.ops[eng].append(dict(fn=_bind(fn), waits=waits, signal=False, dma=slot))
        tok = ('d', slot, cnt + 1)
        self._commit(tok, reads, writes)
        return tok

    def barrier(self):
        toks = []
        for e in ('pe', 'act', 'dve'):
            idx = len(self.ops[e]) - 1
            while idx >= 0 and self.ops[e][idx]['fn'] is None:
                idx -= 1
            if idx >= 0:
                toks.append(('c', e, idx))
        for s in range(N_SP_SLOTS):
            if self.dma_count[s] > 0:
                toks.append(('d', s, self.dma_count[s]))
        for e in ('pe', 'act', 'dve', 'sp'):
            mine = [t for t in toks if not (t[0] == 'c' and t[1] == e)]
            waits = self._reduce(e, mine)
            if waits:
                self.ops[e].append(dict(fn=None, waits=waits, signal=False, dma=None))

    def finish(self, eng='sp'):
        waits = self._reduce(eng, list(self.out_toks))
        self.ops[eng].append(dict(fn=None, waits=waits, signal=False, dma=None))

    def emit(self):
        nc = self.nc
        with ExitStack() as st:
            csem = {e: st.enter_context(nc.semaphore('c_' + e)) for e in self.ENG}
            dsem = [st.enter_context(nc.semaphore('d%d' % i)) for i in range(N_DMA_SLOTS)]
            sigcnt = {}
            for e in self.ENG:
                c = 0
                arr = []
                for o in self.ops[e]:
                    if o['signal']:
                        c += 1
                    arr.append(c)
                sigcnt[e] = arr
            block = st.enter_context(nc.Block())

            def make(e):
                def body(eng):
                    for o in self.ops[e]:
                        for src, v in o['waits']:
                            if src[0] == 'c':
                                eng.wait_ge(csem[src[1]], sigcnt[src[1]][v])
                            else:
                                eng.wait_ge(dsem[src[1]], 16 * v)
                        if o['fn'] is None:
                            continue
                        nm_, a_, k_ = o['fn']
                        ins = getattr(eng, nm_)(*a_, **k_)
                        if o['dma'] is not None:
                            ins.then_inc(dsem[o['dma']], 16)
                        elif o['signal']:
                            ins.then_inc(csem[e], 1)
                return body

            if self.ops['sp']:
                block.sync(make('sp'))
            if self.ops['pe']:
                block.tensor(make('pe'))
            if self.ops['act']:
                block.scalar(make('act'))
            if self.ops['dve']:
                block.vector(make('dve'))
            if self.ops['pool']:
                block.gpsimd(make('pool'))


def fm_layout(L):
    off = {}
    o = 0
    for l in range(L):
        off[('b_in', l)] = o; o += 44
        off[('conv_w', l)] = o; o += 8 * 31
        off[('conv_b', l)] = o; o += 8
        off[('cln_g', l)] = o; o += 8
        off[('cln_b', l)] = o; o += 8
    return off, o


def bc_layout(L):
    off = {}
    o = 0
    off['emb_g'] = o; o += D
    off['emb_b'] = o; o += D
    for l in range(L):
        for nm in ('ln1_g', 'ln1_b', 'ln2_g', 'ln2_b', 'b_pg'):
            off[(nm, l)] = o; o += D
        off[('b_v', l)] = o; o += 256
        off[('b_r', l)] = o; o += 32
        off[('sink', l)] = o; o += 32
    return off, o


def q_perm():
    perm = np.zeros(1024, dtype=np.int64)
    n = 0
    for p in range(2):
        for g in range(4):
            for e in range(2):
                for d in range(64):
                    perm[n] = ((2 * p + e) * 4 + g) * 64 + d
                    n += 1
    return perm


def slope(kv, g):
    hq = kv * 4 + g
    return float(2.0 ** (-8.0 * (hq + 1) / 16.0))


def build_nc(nseq, S, depth=DEPTH, debug=False):
    NT = S // 128
    NB = S // 512
    L = depth
    nc = bass.Bass("TRN2", target_bir_lowering=False)
    fmo, NV = fm_layout(L)
    bco, NBC = bc_layout(L)

    def din(name, shape, dt=F32):
        return nc.dram_tensor(name, list(shape), dt, kind="ExternalInput").ap()

    x_d = din("x", [nseq, S, D])
    p_d = din("p", [L, nseq, S, 256])
    w_in_d = din("w_in", [L, D, IN_COLS])
    w_co_d = din("w_conv_out", [L, D, D])
    w_ao_d = din("w_attn_out", [L, D, D])
    w_o_d = din("w_out", [L, D, D])
    w_r_d = din("w_r", [L, D, 32])
    w1_d = din("w1", [L, 16, D, 512])
    w3_d = din("w3", [L, 16, D, 512])
    w2_d = din("w2", [L, 16, 512, D])
    w_p_d = din("w_p", [L, 256, D])
    w_pg_d = din("w_pg", [L, D, D])
    vfm_d = din("vfm", [128, NV])
    vbc_d = din("vbc", [NBC])
    alibi_d = din("alibi", [2, 2, 128, 6, 512], BF16)
    ident_d = din("ident", [128, 128])
    out_d = nc.dram_tensor("out", [nseq, S, D], F32, kind="ExternalOutput").ap()
    xs_d = nc.dram_tensor("xres", [nseq, S, D], F32, kind="Internal").ap()
    dbg_d = nc.dram_tensor("dbg", [4, nseq, S, D], F32, kind="ExternalOutput").ap() if debug else None

    WORK_BYTES = 42 * 1024

    with ExitStack() as st:
        def sb(name, shape, dt):
            return st.enter_context(nc.sbuf_tensor(name, list(shape), dt))

        xT = sb("xT", [128, KC, S], BF16)
        r2 = sb("r2", [128, 32768 * S // 2048], BF16)
        ring = [sb("ring%d" % i, [128, 4096], BF16) for i in range(6)]
        work = sb("work", [128, WORK_BYTES // 2], BF16)
        ident_f = sb("ident_f", [128, 128], F32)
        ident_b = sb("ident_b", [128, 128], BF16)
        ones_b = sb("ones_b", [128, 128], BF16)
        vfm = sb("vfm_s", [128, NV], F32)
        bq8 = sb("bq8", [128, L * 8], F32)
        gb = sb("gb", [128, 2, D], F32)
        bv_bc = sb("bv_bc", [128, 256], F32)
        br_bc = sb("br_bc", [128, 32], F32)
        esink = sb("esink", [128, 32], F32)
        bpg_h = sb("bpg_h", [1, D], BF16)
        bpg_l = sb("bpg_l", [1, D], BF16)
        comb = sb("comb", [128, NT, 16], F32)
        Mk = [sb("Mk%d" % i, [128, 128], BF16) for i in range(2)]
        sinkL = sb("sinkL", [64, 128], BF16)
        sinkR = [sb("sinkR%d" % i, [64, 512], BF16) for i in range(2)]
        onesf = sb("onesf", [64, 128], F32)
        ps = [st.enter_context(nc.psum_tensor("ps%d" % i, [128, 512], F32)) for i in range(8)]

        half = 16384 * S // 2048
        bufA = r2[:, 0:half].rearrange("p (c s) -> p c s", c=KC)
        bufM = r2[:, half:2 * half].rearrange("p (c s) -> p c s", c=KC)
        acc = r2[:, :].bitcast(F32).rearrange("p (t d) -> p t d", d=D)

        Sx = Sched(nc)

        wstate = {'off': 0}

        def wreset():
            wstate['off'] = 0

        def walloc(shape, dt):
            n = int(np.prod(shape))
            nbytes = n * (4 if dt == F32 else 2)
            nbytes = (nbytes + 63) // 64 * 64
            o = wstate['off']
            assert o + nbytes <= WORK_BYTES, ("work overflow", o, nbytes)
            wstate['off'] = o + nbytes
            v = work[:, o // 2:(o + nbytes) // 2]
            if dt == F32:
                v = v.bitcast(F32)
            v = v[:, 0:n]
            if len(shape) == 2:
                v = v.rearrange("p (a b) -> p a b", a=shape[0])
            elif len(shape) == 3:
                v = v.rearrange("p (a b c) -> p a b c", a=shape[0], b=shape[1])
            return v

        rstate = {'n': 0}

        def wpanel(src_ap, kc, ncols):
            slot = rstate['n'] % 6
            rstate['n'] += 1
            dst = ring[slot][:, 0:kc * ncols].rearrange("p (k n) -> p k n", k=kc)
            src = src_ap.rearrange("(k p) n -> p k n", p=128)
            key = ('W', slot)
            Sx.dma('pool', lambda e: e.dma_start(out=dst, in_=src), writes=[key])
            return dst, key

        def mm(out, lhsT, rhs, start, stop, reads, writes):
            Sx.op('pe', lambda e: e.matmul(out, lhsT=lhsT, rhs=rhs, start=start, stop=stop),
                  reads=reads, writes=writes)

        def xT_keys(tb):
            return [('xT', tb * 4 + i) for i in range(4)]

        Sx.dma('sp', lambda e: e.dma_start(out=ident_f[:], in_=ident_d[:, :]), writes=['ident_f'])
        Sx.dma('sp', lambda e: e.dma_start(out=vfm[:], in_=vfm_d[:, :]), writes=['vfm'])
        Sx.op('dve', lambda e: e.tensor_copy(out=ident_b[:], in_=ident_f[:]), reads=['ident_f'], writes=['ident_b'])
        Sx.op('dve', lambda e: e.memset(ones_b[:], 1.0), writes=['ones_b'])
        Sx.op('dve', lambda e: e.memset(onesf[:], 1.0), writes=['onesf'])
        Sx.op('dve', lambda e: e.memset(sinkL[:], 0.0), writes=['sinkL'])
        Sx.op('dve', lambda e: e.memset(sinkL[0:1, 0:64], 1.0), writes=['sinkL'])
        Sx.op('dve', lambda e: e.memset(sinkL[32:33, 64:128], 1.0), writes=['sinkL'])
        for i_ in range(2):
            Sx.op('dve', lambda e: e.memset(Mk[i_][:], 0.0), writes=['Mk'])
            Sx.op('dve', lambda e: e.memset(Mk[i_][:, i_ * 64:(i_ + 1) * 64], 1.0), writes=['Mk'])
            Sx.op('dve', lambda e: e.memset(sinkR[i_][:], 0.0), writes=[('sinkR', i_)])
        for l in range(L):
            o = fmo[('b_in', l)] + 16
            Sx.op('dve', lambda e, o=o, l=l: e.tensor_scalar(out=bq8[:, l * 8:(l + 1) * 8], in0=vfm[:, o:o + 8],
                                                              scalar1=0.125, scalar2=None, op0=ALU.mult),
                  reads=['vfm'], writes=[('bq8', l)])

        def load_gb(goff, boff):
            Sx.dma('sp', lambda e: e.dma_start(out=gb[:, 0, :], in_=vbc_d[goff:goff + D].partition_broadcast(128)),
                   writes=['gb0'])
            Sx.dma('sp', lambda e: e.dma_start(out=gb[:, 1, :], in_=vbc_d[boff:boff + D].partition_broadcast(128)),
                   writes=['gb1'])

        NLN = 6

        def ln_phase(ln, pre, prefetch, dst, write_xT, final, nt, pre_pe=None):
            info = {}

            def st0(tt):
                z, zkeys = pre(tt)
                info[tt] = (z, zkeys)
                b = tt % NLN
                stats, mv, rs = ln['stats'][b], ln['mv'][b], ln['rs'][b]
                kb = 'ln%d' % b
                for h in range(2):
                    Sx.op('dve', lambda e: e.bn_stats(out=stats[:, h * 6:(h + 1) * 6], in_=z[:, h * 512:(h + 1) * 512]),
                          reads=zkeys, writes=[(kb, 'st', h)])
                Sx.op('dve', lambda e: e.bn_aggr(out=mv[:, :], in_=stats[:, :]),
                      reads=[(kb, 'st', 0), (kb, 'st', 1)], writes=[(kb, 'mv')])
                Sx.op('dve', lambda e: e.tensor_scalar(out=rs[:, 0:1], in0=mv[:, 1:2], scalar1=LN_EPS, scalar2=None,
                                                       op0=ALU.add),
                      reads=[(kb, 'mv')], writes=[(kb, 'rs0')])

            def st1(tt):
                b = tt % NLN
                rs = ln['rs'][b]
                kb = 'ln%d' % b
                Sx.op('act', lambda e: e.activation(out=rs[:, 0:1], in_=rs[:, 0:1], func=AF.Sqrt),
                      reads=[(kb, 'rs0')], writes=[(kb, 'rs0')])

            def st2(tt):
                b = tt % NLN
                mv, rs = ln['mv'][b], ln['rs'][b]
                kb = 'ln%d' % b
                Sx.op('dve', lambda e: e.reciprocal(out=rs[:, 0:1], in_=rs[:, 0:1]),
                      reads=[(kb, 'rs0')], writes=[(kb, 'rs0')])
                Sx.op('dve', lambda e: e.scalar_tensor_tensor(out=rs[:, 1:2], in0=mv[:, 0:1], scalar=-1.0, in1=rs[:, 0:1],
                                                              op0=ALU.mult, op1=ALU.mult),
                      reads=[(kb, 'mv'), (kb, 'rs0')], writes=[(kb, 'rs1')])

            def st3(tt):
                b = tt % NLN
                rs = ln['rs'][b]
                kb = 'ln%d' % b
                z, zkeys = info[tt]
                Sx.op('act', lambda e: e.activation(out=z, in_=z, func=AF.Identity, bias=rs[:, 1:2], scale=rs[:, 0:1]),
                      reads=zkeys + [(kb, 'rs0'), (kb, 'rs1')], writes=zkeys)

            def st4(tt):
                z, zkeys = info[tt]
                xb = ln['xb'][tt % 4]
                xk = ('lnxb', tt % 4)
                pbank = 4 + tt % 4
                dst_d, dst_key = dst(tt)
                Sx.op('dve', lambda e: e.tensor_tensor(out=z, in0=z, in1=gb[:, 0, :], op=ALU.mult),
                      reads=zkeys + ['gb0'], writes=zkeys)
                Sx.op('dve', lambda e: e.tensor_tensor(out=z, in0=z, in1=gb[:, 1, :], op=ALU.add),
                      reads=zkeys + ['gb1'], writes=zkeys)
                t = Sx.dma('sp', lambda e: e.dma_start(out=dst_d, in_=z), reads=zkeys, writes=[dst_key])
                if final:
                    Sx.out_toks.append(t)
                if write_xT:
                    Sx.op('act', lambda e: e.activation(out=xb[:, :], in_=z, func=AF.Identity),
                          reads=zkeys, writes=[xk])
                    pview = ps[pbank][:, :].bitcast(BF16).rearrange("p (k n) -> p k n", k=KC)
                    for k in range(KC):
                        Sx.op('pe', lambda e: e.transpose(out=pview[:, k, :], in_=xb[:, k * 128:(k + 1) * 128],
                                                          identity=ident_b[:]),
                              reads=[xk, 'ident_b'], writes=[('ps', pbank)])

            def st5(tt):
                if write_xT:
                    pbank = 4 + tt % 4
                    pview = ps[pbank][:, :].bitcast(BF16).rearrange("p (k n) -> p k n", k=KC)
                    Sx.op('act', lambda e: e.activation(out=xT[:, :, tt * 128:(tt + 1) * 128], in_=pview, func=AF.Identity),
                          reads=[('ps', pbank)], writes=[('xT', tt)])

            stages = [st0, st1, st2, st3, st4, st5]
            if prefetch is not None:
                for tt in range(min(2, nt)):
                    prefetch(tt)
            if pre_pe is not None:
                pre_pe(0)
            for step in range(nt + len(stages) - 1):
                if pre_pe is not None and step + 1 < nt:
                    pre_pe(step + 1)
                for k, f in enumerate(stages):
                    tt = step - k
                    if 0 <= tt < nt:
                        f(tt)
                if prefetch is not None and step + 2 < nt:
                    prefetch(step + 2)

        def ln_bufs():
            return dict(stats=[walloc([12], F32) for _ in range(NLN)],
                        mv=[walloc([2], F32) for _ in range(NLN)],
                        rs=[walloc([2], F32) for _ in range(NLN)],
                        xb=[walloc([D], BF16) for _ in range(4)])

        for s in range(nseq):
            Sx.barrier()
            wreset()
            ln = ln_bufs()
            zin = [walloc([D], F32) for _ in range(NLN)]
            load_gb(bco['emb_g'], bco['emb_b'])

            def emb_load(tt):
                b = tt % NLN
                Sx.dma('sp', lambda e: e.dma_start(out=zin[b][:, :], in_=x_d[s, tt * 128:(tt + 1) * 128, :]),
                       writes=[('zin', b)])
            ln_phase(ln, lambda tt: (zin[tt % NLN][:, :], [('zin', tt % NLN)]), emb_load,
                     lambda tt: (xs_d[s, tt * 128:(tt + 1) * 128, :], ('xs', tt)), True, False, NT)
            if debug:
                t_ = Sx.dma('sp', lambda e: e.dma_start(out=dbg_d[0, s], in_=xs_d[s]), reads=[('xs', tt) for tt in range(NT)])
                Sx.out_toks.append(t_)
            for l in range(L):
                if debug and l == 1:
                    t_ = Sx.dma('sp', lambda e: e.dma_start(out=dbg_d[2, s], in_=xs_d[s]), reads=[('xs', tt) for tt in range(NT)])
                    Sx.out_toks.append(t_)
                bo = fmo[('b_in', l)]
                cwo = fmo[('conv_w', l)]
                cbo = fmo[('conv_b', l)]
                cgo = fmo[('cln_g', l)]
                cbb = fmo[('cln_b', l)]
                last_layer = (l == L - 1)

                Sx.barrier()
                wreset()
                ubuf = [walloc([S + 30], BF16) for _ in range(2)]
                diag = [walloc([31, 128], BF16) for _ in range(2)]
                sgb = [walloc([512], F32) for _ in range(2)]
                ysq = [walloc([512], BF16) for _ in range(2)]
                mean_t = [walloc([512], F32) for _ in range(2)]
                rstd_t = [walloc([512], F32) for _ in range(2)]
                t1b = [walloc([512], F32) for _ in range(2)]
                for b in range(2):
                    Sx.op('dve', lambda e, b=b: e.memset(ubuf[b][:, 0:15], 0.0), writes=[('u', b)])
                    Sx.op('dve', lambda e, b=b: e.memset(ubuf[b][:, S + 15:S + 30], 0.0), writes=[('u', b)])
                cstate = {'cnt': 0, 'W': None}

                def glu(c):
                    j, cc = c // 4, c % 4
                    if cc == 0:
                        Wa, ka = wpanel(w_in_d[l, :, j * 512:(j + 1) * 512], KC, 512)
                        Wg, kg = wpanel(w_in_d[l, :, 1024 + j * 512:1024 + (j + 1) * 512], KC, 512)
                        cstate['W'] = (Wa, ka, Wg, kg)
                    Wa, ka, Wg, kg = cstate['W']
                    ub = c % 2
                    for tb in range(NB):
                        cnt = cstate['cnt']
                        pa, pg = (cnt % 2), 2 + (cnt % 2)
                        sg = sgb[cnt % 2]
                        skey = ('sg', cnt % 2)
                        cstate['cnt'] += 1
                        for k in range(KC):
                            mm(ps[pa][:, :], Wa[:, k, cc * 128:(cc + 1) * 128], xT[:, k, tb * 512:(tb + 1) * 512],
                               k == 0, k == KC - 1, [ka] + xT_keys(tb), [('ps', pa)])
                        for k in range(KC):
                            mm(ps[pg][:, :], Wg[:, k, cc * 128:(cc + 1) * 128], xT[:, k, tb * 512:(tb + 1) * 512],
                               k == 0, k == KC - 1, [kg] + xT_keys(tb), [('ps', pg)])
                        Sx.op('act', lambda e: e.activation(
                            out=sg[:, :], in_=ps[pg][:, :], func=AF.Sigmoid, bias=vfm[:, bo + 8 + c:bo + 9 + c]),
                            reads=[('ps', pg), 'vfm'], writes=[skey])
                        Sx.op('dve', lambda e: e.scalar_tensor_tensor(
                            out=ubuf[ub][:, 15 + tb * 512:15 + (tb + 1) * 512], in0=ps[pa][:, :],
                            scalar=vfm[:, bo + c:bo + c + 1], in1=sg[:, :], op0=ALU.add, op1=ALU.mult),
                            reads=[('ps', pa), skey, 'vfm'], writes=[('u', ub)])
                    Sx.op('dve', lambda e: e.tensor_tensor(
                        out=diag[ub][:, :, :],
                        in0=ident_b[:, :].unsqueeze(1).to_broadcast([128, 31, 128]),
                        in1=vfm[:, cwo + c * 31:cwo + (c + 1) * 31].unsqueeze(2).to_broadcast([128, 31, 128]),
                        op=ALU.mult), reads=['ident_b', 'vfm'], writes=[('diag', ub)])

                def conv(c):
                    ub = c % 2
                    for tb in range(NB):
                        pc = 4 + (tb % 2)
                        for k in range(31):
                            mm(ps[pc][:, :], diag[ub][:, k, :], ubuf[ub][:, tb * 512 + k:tb * 512 + k + 512],
                               k == 0, k == 30, [('diag', ub), ('u', ub)], [('ps', pc)])
                        Sx.op('act', lambda e: e.activation(
                            out=bufA[:, c, tb * 512:(tb + 1) * 512], in_=ps[pc][:, :], func=AF.Identity,
                            bias=vfm[:, cbo + c:cbo + c + 1]),
                            reads=[('ps', pc), 'vfm'], writes=[('A', c, tb * 4 + i, e2) for i in range(4) for e2 in range(2)])

                for c in range(KC + 1):
                    if c < KC:
                        glu(c)
                    if c >= 1:
                        conv(c - 1)
                def akeys(c, tb):
                    return [('A', c, tb * 4 + i, e2) for i in range(4) for e2 in range(2)]

                def cstats_a(tb):
                    for c in range(KC):
                        yq = ysq[c % 2]
                        Sx.op('act', lambda e: e.activation(
                            out=yq[:, :], in_=bufA[:, c, tb * 512:(tb + 1) * 512], func=AF.Square),
                            reads=akeys(c, tb), writes=[('ysq', c % 2)])
                        mm(ps[6][:, :], ones_b[:, :], bufA[:, c, tb * 512:(tb + 1) * 512], c == 0, c == KC - 1,
                           akeys(c, tb) + ['ones_b'], [('ps', 6)])
                        mm(ps[7][:, :], ones_b[:, :], yq[:, :], c == 0, c == KC - 1,
                           [('ysq', c % 2), 'ones_b'], [('ps', 7)])

                def cstats_c(tb):
                    mt, rt_ = mean_t[tb % 2], rstd_t[tb % 2]
                    mk_, rk_ = ('mean_t', tb % 2), ('rstd_t', tb % 2)
                    Sx.op('dve', lambda e: e.tensor_scalar(out=mt[:, :], in0=ps[6][:, :], scalar1=1.0 / D, scalar2=None,
                                                           op0=ALU.mult), reads=[('ps', 6)], writes=[mk_])
                    Sx.op('dve', lambda e: e.tensor_tensor(out=rt_[:, :], in0=mt[:, :], in1=mt[:, :], op=ALU.mult),
                          reads=[mk_], writes=[rk_])
                    Sx.op('dve', lambda e: e.scalar_tensor_tensor(out=rt_[:, :], in0=ps[7][:, :], scalar=1.0 / D,
                                                                  in1=rt_[:, :], op0=ALU.mult, op1=ALU.subtract),
                          reads=[('ps', 7), rk_], writes=[rk_])
                    Sx.op('dve', lambda e: e.tensor_scalar(out=rt_[:, :], in0=rt_[:, :], scalar1=LN_EPS, scalar2=None,
                                                           op0=ALU.add), reads=[rk_], writes=[rk_])
                    Sx.op('act', lambda e: e.activation(out=rt_[:, :], in_=rt_[:, :], func=AF.Sqrt),
                          reads=[rk_], writes=[rk_])
                    Sx.op('dve', lambda e: e.reciprocal(out=rt_[:, :], in_=rt_[:, :]),
                          reads=[rk_], writes=[rk_])

                def cnorm(tb):
                    mt, rt_ = mean_t[tb % 2], rstd_t[tb % 2]
                    mk_, rk_ = ('mean_t', tb % 2), ('rstd_t', tb % 2)
                    for c in range(KC):
                        t1 = t1b[c % 2]
                        Sx.op('dve', lambda e: e.tensor_tensor(
                            out=t1[:, :], in0=bufA[:, c, tb * 512:(tb + 1) * 512], in1=mt[:, :], op=ALU.subtract),
                            reads=akeys(c, tb) + [mk_], writes=[('t1', c % 2)])
                        Sx.op('dve', lambda e: e.tensor_tensor(out=t1[:, :], in0=t1[:, :], in1=rt_[:, :], op=ALU.mult),
                              reads=[('t1', c % 2), rk_], writes=[('t1', c % 2)])
                        Sx.op('act', lambda e: e.activation(
                            out=bufA[:, c, tb * 512:(tb + 1) * 512], in_=t1[:, :], func=AF.Silu,
                            bias=vfm[:, cbb + c:cbb + c + 1], scale=vfm[:, cgo + c:cgo + c + 1]),
                            reads=[('t1', c % 2), 'vfm'], writes=akeys(c, tb))

                cstats_a(0)
                cstats_c(0)
                for tb in range(NB):
                    if tb + 1 < NB:
                        cstats_a(tb + 1)
                    cnorm(tb)
                    if tb + 1 < NB:
                        cstats_c(tb + 1)

                def gated_branch(w_src, gate_col0, gate_b0, accumulate):
                    cnt2 = 0
                    for j in range(2):
                        Wy, ky = wpanel(w_src[:, j * 512:(j + 1) * 512], KC, 512)
                        Wg2, kg2 = wpanel(w_in_d[l, :, gate_col0 + j * 512:gate_col0 + (j + 1) * 512], KC, 512)
                        for mmi in range(4):
                            m = 4 * j + mmi
                            for tb in range(NB):
                                py, pg2 = (cnt2 % 2), 2 + (cnt2 % 2)
                                sg = sgb[cnt2 % 2]
                                skey = ('sg', cnt2 % 2)
                                tmp = t1b[cnt2 % 2]
                                tkey = ('t1', cnt2 % 2)
                                cnt2 += 1
                                ak = [('A', k2, tb * 4 + i, e2) for k2 in range(KC) for i in range(4) for e2 in range(2)]
                                for k in range(KC):
                                    mm(ps[py][:, :], Wy[:, k, mmi * 128:(mmi + 1) * 128], bufA[:, k, tb * 512:(tb + 1) * 512],
                                       k == 0, k == KC - 1, [ky] + ak, [('ps', py)])
                                for k in range(KC):
                                    mm(ps[pg2][:, :], Wg2[:, k, mmi * 128:(mmi + 1) * 128], xT[:, k, tb * 512:(tb + 1) * 512],
                                       k == 0, k == KC - 1, [kg2] + xT_keys(tb), [('ps', pg2)])
                                Sx.op('act', lambda e, pg2=pg2, sg=sg, m=m: e.activation(
                                    out=sg[:, :], in_=ps[pg2][:, :], func=AF.Sigmoid,
                                    bias=vfm[:, bo + gate_b0 + m:bo + gate_b0 + m + 1]),
                                    reads=[('ps', pg2), 'vfm'], writes=[skey])
                                if not accumulate:
                                    Sx.op('dve', lambda e, py=py, sg=sg, m=m, tb=tb: e.tensor_tensor(
                                        out=bufM[:, m, tb * 512:(tb + 1) * 512], in0=ps[py][:, :], in1=sg[:, :], op=ALU.mult),
                                        reads=[('ps', py), skey], writes=[('M', m, tb)])
                                else:
                                    Sx.op('dve', lambda e, py=py, sg=sg, tmp=tmp: e.tensor_tensor(
                                        out=tmp[:, :], in0=ps[py][:, :], in1=sg[:, :], op=ALU.mult),
                                        reads=[('ps', py), skey], writes=[tkey])
                                    Sx.op('dve', lambda e, tmp=tmp, m=m, tb=tb: e.tensor_tensor(
                                        out=bufM[:, m, tb * 512:(tb + 1) * 512], in0=tmp[:, :],
                                        in1=bufM[:, m, tb * 512:(tb + 1) * 512], op=ALU.add),
                                        reads=[tkey, ('M', m, tb)], writes=[('M', m, tb)])

                gated_branch(w_co_d[l], 3584, 28, False)

                Sx.barrier()
                wreset()
                KTm = walloc([2, S], BF16)
                Vm = walloc([NT, 2, 128], BF16)
                biasH = walloc([6, 512], BF16)
                biasL = walloc([6, 512], BF16)
                PT = [walloc([512], BF16) for _ in range(6)]
                rec = [walloc([512], F32) for _ in range(2)]
                Sx.dma('sp', lambda e: e.dma_start(out=bv_bc[:, :], in_=vbc_d[bco[('b_v', l)]:bco[('b_v', l)] + 256].partition_broadcast(128)),
                       writes=['bv_bc'])
                Sx.dma('sp', lambda e: e.dma_start(out=esink[:, :], in_=vbc_d[bco[('sink', l)]:bco[('sink', l)] + 32].partition_broadcast(128)),
                       writes=['esink'])
                Sx.op('act', lambda e: e.activation(out=esink[:, :], in_=esink[:, :], func=AF.Exp),
                      reads=['esink'], writes=['esink'])
                for p in range(2):
                    for e2 in range(2):
                        for g in range(4):
                            hq = (2 * p + e2) * 4 + g
                            r = 32 * e2
                            Sx.op('dve', lambda e: e.tensor_scalar(
                                out=sinkR[p][r:r + 1, g * 128:(g + 1) * 128], in0=onesf[r:r + 1, 0:128],
                                scalar1=esink[r:r + 1, hq:hq + 1], scalar2=None, op0=ALU.mult),
                                reads=['esink', 'onesf'], writes=[('sinkR', p)])
                Sx.op('dve', lambda e: e.memset(Vm[:, :, :, :], 0.0), writes=[('V', tt) for tt in range(NT)])
                Sx.op('dve', lambda e: e.memset(KTm[:, :, :], 0.0), writes=[('KT', tt) for tt in range(NT)])
                Wkv, kkv = wpanel(w_in_d[l, :, 3072:3584], KC, 512)
                cnt = 0
                for j in range(2):
                    Wq, kq = wpanel(w_in_d[l, :, 2048 + j * 512:2048 + (j + 1) * 512], KC, 512)
                    for cc in range(4):
                        qc = 4 * j + cc
                        for tb in range(NB):
                            pb = 4 + cnt % 2
                            cnt += 1
                            for k in range(KC):
                                mm(ps[pb][:, :], Wq[:, k, cc * 128:(cc + 1) * 128], xT[:, k, tb * 512:(tb + 1) * 512],
                                   k == 0, k == KC - 1, [kq] + xT_keys(tb), [('ps', pb)])
                            Sx.op('act', lambda e: e.activation(
                                out=bufA[:, qc, tb * 512:(tb + 1) * 512], in_=ps[pb][:, :], func=AF.Identity,
                                bias=bq8[:, l * 8 + qc:l * 8 + qc + 1], scale=0.125),
                                reads=[('ps', pb), ('bq8', l)],
                                writes=[('A', qc, tb * 4 + i, e2) for i in range(4) for e2 in range(2)])
                scnt = 0
                ocnt = 0
                pcnt = 0
                for p in range(2):
                    for tb in range(NB):
                        pb = 6 + tb % 2
                        for k in range(KC):
                            mm(ps[pb][:, :], Wkv[:, k, p * 128:(p + 1) * 128], xT[:, k, tb * 512:(tb + 1) * 512],
                               k == 0, k == KC - 1, [kkv] + xT_keys(tb), [('ps', pb)])
                        for ee in range(2):
                            Sx.op('act', lambda e: e.activation(
                                out=KTm[ee * 64:(ee + 1) * 64, ee, tb * 512:(tb + 1) * 512], in_=ps[pb][ee * 64:(ee + 1) * 64, :],
                                func=AF.Identity, bias=vfm[ee * 64:(ee + 1) * 64, bo + 24 + p:bo + 25 + p]),
                                reads=[('ps', pb), 'vfm'], writes=[('KT', tb * 4 + i) for i in range(4)])
                    for tt in range(NT):
                        pb = 6 + tt % 2
                        for k in range(KC):
                            mm(ps[pb][:, 0:128], xT[:, k, tt * 128:(tt + 1) * 128], Wkv[:, k, 256 + p * 128:256 + (p + 1) * 128],
                               k == 0, k == KC - 1, [kkv, ('xT', tt)], [('ps', pb)])
                        for e2 in range(2):
                            Sx.op('dve', lambda e: e.tensor_tensor(
                                out=Vm[:, tt, e2, e2 * 64:(e2 + 1) * 64], in0=ps[pb][:, e2 * 64:(e2 + 1) * 64],
                                in1=bv_bc[:, p * 128 + e2 * 64:p * 128 + (e2 + 1) * 64], op=ALU.add),
                                reads=[('ps', pb), 'bv_bc'], writes=[('V', tt)])
                    Sx.dma('sp', lambda e: e.dma_start(out=biasH[:, :, :], in_=alibi_d[p, 0]), writes=['biasH'])
                    Sx.dma('sp', lambda e: e.dma_start(out=biasL[:, :, :], in_=alibi_d[p, 1]), writes=['biasL'])
                    steps = []
                    for i in range(NT):
                        js = [j for j in (i - 1, i, i + 1) if 0 <= j < NT]
                        nmm = 2 * len(js)
                        c = 0
                        for e2 in range(2):
                            for j in js:
                                steps.append(dict(i=i, e2=e2, j=j, c=c, nmm=nmm, u=ocnt))
                                c += 1
                        ocnt += 1

                    def emit_S(st_):
                        i, e2, j = st_['i'], st_['e2'], st_['j']
                        R0, R1 = e2 * 64, e2 * 64 + 64
                        jrel = j - i + 1
                        pS = st_['t'] % 3
                        Pt = PT[st_['t'] % 6]
                        pk = ('PT', st_['t'] % 6)
                        qkeys = [('A', p * 4 + g, i, ee) for g in range(4) for ee in range(2)]
                        mm(ps[pS][:, :].rearrange("p (g n) -> p g n", g=4), KTm[:, e2, j * 128:(j + 1) * 128],
                           bufA[:, p * 4:(p + 1) * 4, i * 128:(i + 1) * 128],
                           True, False, [('KT', j)] + qkeys, [('ps', pS)])
                        mm(ps[pS][:, :], ident_b[:, :], biasH[:, e2 * 3 + jrel, :], False, False,
                           ['ident_b', 'biasH'], [('ps', pS)])
                        mm(ps[pS][:, :], ident_b[:, :], biasL[:, e2 * 3 + jrel, :], False, True,
                           ['ident_b', 'biasL'], [('ps', pS)])
                        Sx.op('act', lambda e: e.activation(out=Pt[:, :], in_=ps[pS][:, :], func=AF.Exp),
                              reads=[('ps', pS)], writes=[pk])

                    def emit_PV(st_):
                        i, e2, j, c, nmm, u = st_['i'], st_['e2'], st_['j'], st_['c'], st_['nmm'], st_['u']
                        Pt = PT[st_['t'] % 6]
                        pk = ('PT', st_['t'] % 6)
                        pO = 3 + (u % 2)
                        pD = 5 + (u % 2)
                        mm(ps[pO][:, :], Vm[:, j, e2, :], Pt[:, :], c == 0, c == nmm - 1, [('V', j), pk], [('ps', pO)])
                        mm(ps[pD][:, :], Mk[e2][:, :], Pt[:, :], c == 0, False, ['Mk', pk], [('ps', pD)])
                        if c == nmm - 1:
                            rc = rec[u % 2]
                            rk = ('rec', u % 2)
                            qall = [('A', p * 4 + g, i, ee) for g in range(4) for ee in range(2)]
                            mm(ps[pD][:, :], sinkL[0:33, :], sinkR[p][0:33, :], False, True, ['sinkL', ('sinkR', p)], [('ps', pD)])
                            Sx.op('dve', lambda e: e.reciprocal(out=rc[:, :], in_=ps[pD][:, :]), reads=[('ps', pD)], writes=[rk])
                            Sx.op('dve', lambda e: e.tensor_tensor(
                                out=bufA[:, p * 4:(p + 1) * 4, i * 128:(i + 1) * 128],
                                in0=ps[pO][:, :].rearrange("p (g n) -> p g n", g=4),
                                in1=rc[:, :].rearrange("p (g n) -> p g n", g=4), op=ALU.mult),
                                reads=[('ps', pO), rk], writes=qall)

                    LA = 2
                    for t_, st_ in enumerate(steps):
                        st_['t'] = scnt + t_
                    scnt += len(steps)
                    for t_ in range(len(steps) + LA):
                        if t_ < len(steps):
                            emit_S(steps[t_])
                        if t_ - LA >= 0:
                            emit_PV(steps[t_ - LA])

                Sx.barrier()
                wreset()
                sgb = [walloc([512], F32) for _ in range(2)]
                t1b = [walloc([512], F32) for _ in range(2)]
                gated_branch(w_ao_d[l], 4608, 36, True)

                Sx.barrier()
                wreset()
                ln = ln_bufs()
                xr = [walloc([D], F32) for _ in range(NLN)]
                load_gb(bco[('ln1_g', l)], bco[('ln1_b', l)])
                Wo = []
                for j in range(2):
                    Wo.append(wpanel(w_o_d[l, :, j * 512:(j + 1) * 512], KC, 512))

                def xr_load(tt):
                    b = tt % NLN
                    Sx.dma('sp', lambda e: e.dma_start(out=xr[b][:, :], in_=xs_d[s, tt * 128:(tt + 1) * 128, :]),
                           reads=[('xs', tt)], writes=[('xr', b)])

                def c_pe(tt):
                    mkeys = [('M', m, tt // 4) for m in range(KC)]
                    for j in range(2):
                        pb = 2 * (tt % 2) + j
                        for k in range(KC):
                            mm(ps[pb][:, :], bufM[:, k, tt * 128:(tt + 1) * 128], Wo[j][0][:, k, :],
                               k == 0, k == KC - 1, [Wo[j][1]] + mkeys, [('ps', pb)])

                def c_pre(tt):
                    b = tt % NLN
                    for j in range(2):
                        pb = 2 * (tt % 2) + j
                        Sx.op('dve', lambda e: e.scalar_tensor_tensor(
                            out=xr[b][:, j * 512:(j + 1) * 512], in0=xr[b][:, j * 512:(j + 1) * 512], scalar=DN_ALPHA,
                            in1=ps[pb][:, :], op0=ALU.mult, op1=ALU.add),
                            reads=[('ps', pb), ('xr', b)], writes=[('xr', b)])
                    return xr[b][:, :], [('xr', b)]

                ln_phase(ln, c_pre, xr_load, lambda tt: (xs_d[s, tt * 128:(tt + 1) * 128, :], ('xs', tt)), True, False, NT, pre_pe=c_pe)

                if debug:
                    t_ = Sx.dma('sp', lambda e, l=l: e.dma_start(out=dbg_d[1 + 2 * l, s], in_=xs_d[s]), reads=[('xs', tt) for tt in range(NT)])
                    Sx.out_toks.append(t_)
                Sx.barrier()
                wreset()
                lgall = walloc([NT, 32], F32)
                rA = walloc([NT, 16], F32)
                rB = walloc([NT, 16], F32)
                rC = walloc([NT, 16], F32)
                rS = walloc([12, NT], F32)
                rG = walloc([3, NT, 4], F32)
                pin = [walloc([256], F32) for _ in range(4)]
                pTall = walloc([2, S], BF16)
                sgp = [walloc([512], F32) for _ in range(2)]
                tpb = [walloc([512], F32) for _ in range(2)]
                hT = [walloc([4, 512], BF16) for _ in range(2)]
                s1b = [walloc([512], F32) for _ in range(2)]

                def p_load(tt):
                    Sx.dma('sp', lambda e: e.dma_start(out=pin[tt % 4][:, :], in_=p_d[l, s, tt * 128:(tt + 1) * 128, :]),
                           writes=[('pin', tt % 4)])
                for tt in range(min(4, NT)):
                    p_load(tt)
                for tt in range(NT):
                    Sx.dma('sp', lambda e: e.dma_start(out=acc[:, tt, :], in_=xs_d[s, tt * 128:(tt + 1) * 128, :]),
                           reads=[('xs', tt)], writes=[('acc', tt, 0), ('acc', tt, 1)])
                Sx.dma('sp', lambda e: e.dma_start(out=br_bc[:, :], in_=vbc_d[bco[('b_r', l)]:bco[('b_r', l)] + 32].partition_broadcast(128)),
                       writes=['br_bc'])
                bpg_f = gb[0:1, 0, :]
                bpg_t = gb[0:1, 1, :]
                Sx.dma('sp', lambda e: e.dma_start(out=bpg_f, in_=vbc_d[bco[('b_pg', l)]:bco[('b_pg', l)] + D].partition_broadcast(1)),
                       writes=['gb0'])
                Sx.op('dve', lambda e: e.tensor_copy(out=bpg_h[:, :], in_=bpg_f), reads=['gb0'], writes=['bpg_h'])
                Sx.op('dve', lambda e: e.tensor_copy(out=bpg_t, in_=bpg_h[:, :]), reads=['bpg_h'], writes=['gb1'])
                Sx.op('dve', lambda e: e.tensor_tensor(out=bpg_t, in0=bpg_f, in1=bpg_t, op=ALU.subtract),
                      reads=['gb0', 'gb1'], writes=['gb1'])
                Sx.op('dve', lambda e: e.tensor_copy(out=bpg_l[:, :], in_=bpg_t), reads=['gb1'], writes=['bpg_l'])
                Wr, kr = wpanel(w_r_d[l], KC, 32)
                for tt in range(NT):
                    for k in range(KC):
                        mm(ps[0][:, tt * 32:(tt + 1) * 32], xT[:, k, tt * 128:(tt + 1) * 128], Wr[:, k, :], k == 0, k == KC - 1,
                           [kr, ('xT', tt)], [('ps', 0)])
                for tt in range(NT):
                    pb = 6 + tt % 2
                    for k in range(2):
                        Sx.op('pe', lambda e: e.transpose(out=ps[pb][:, k * 128:(k + 1) * 128], in_=pin[tt % 4][:, k * 128:(k + 1) * 128],
                                                          identity=ident_f[:]),
                              reads=[('pin', tt % 4), 'ident_f'], writes=[('ps', pb)])
                    Sx.op('act', lambda e: e.activation(out=pTall[:, :, tt * 128:(tt + 1) * 128],
                                                        in_=ps[pb][:, 0:256].rearrange("p (k n) -> p k n", k=2), func=AF.Identity),
                          reads=[('ps', pb)], writes=[('pT', tt)])
                    if tt + 4 < NT:
                        p_load(tt + 4)
                V = Sx.op
                RK = ['rt']

                def bc(ap2, n):
                    return ap2.unsqueeze(2).to_broadcast([128, NT, n])
                gsh, gmk, pen = rG[:, 0, :, :], rG[:, 1, :, :], rG[:, 2, :, :]
                sc = lambda i: rS[:, i, :]
                em4 = rA[:, :, :].rearrange("p t (g k) -> p t g k", g=4)
                V('dve', lambda e: e.tensor_tensor(out=lgall[:, :, :], in0=ps[0][:, 0:NT * 32].rearrange("p (t n) -> p t n", n=32),
                                                   in1=br_bc[:, :].unsqueeze(1).to_broadcast([128, NT, 32]), op=ALU.add),
                  reads=[('ps', 0), 'br_bc'], writes=RK)
                V('dve', lambda e: e.tensor_reduce(out=sc(0), in_=lgall[:, :, 0:4], axis=AX.X, op=ALU.max), reads=RK, writes=RK)
                V('dve', lambda e: e.tensor_tensor(out=gsh, in0=lgall[:, :, 0:4], in1=bc(sc(0), 4), op=ALU.subtract), reads=RK, writes=RK)
                V('dve', lambda e: e.tensor_scalar(out=gmk, in0=gsh, scalar1=0.0, scalar2=None, op0=ALU.is_ge), reads=RK, writes=RK)
                V('dve', lambda e: e.tensor_scalar(out=pen, in0=gmk, scalar1=-1.0, scalar2=BIG, op0=ALU.add, op1=ALU.mult), reads=RK, writes=RK)
                V('act', lambda e: e.activation(out=gsh, in_=gsh, func=AF.Exp), reads=RK, writes=RK)
                V('dve', lambda e: e.tensor_reduce(out=sc(1), in_=gsh, axis=AX.X, op=ALU.add), reads=RK, writes=RK)
                V('dve', lambda e: e.reciprocal(out=sc(2), in_=sc(1)), reads=RK, writes=RK)
                V('dve', lambda e: e.tensor_tensor(out=em4, in0=lgall[:, :, 4:20].rearrange("p t (g k) -> p t g k", g=4),
                                                   in1=pen.unsqueeze(3).to_broadcast([128, NT, 4, 4]), op=ALU.add), reads=RK, writes=RK)
                V('dve', lambda e: e.tensor_reduce(out=sc(3), in_=rA[:, :, :], axis=AX.X, op=ALU.max), reads=RK, writes=RK)
                V('dve', lambda e: e.tensor_tensor(out=rB[:, :, :], in0=rA[:, :, :], in1=bc(sc(3), 16), op=ALU.is_ge), reads=RK, writes=RK)
                V('dve', lambda e: e.scalar_tensor_tensor(out=rA[:, :, :], in0=rB[:, :, :], scalar=-BIG, in1=rA[:, :, :], op0=ALU.mult, op1=ALU.add),
                  reads=RK, writes=RK)
                V('dve', lambda e: e.tensor_reduce(out=sc(4), in_=rA[:, :, :], axis=AX.X, op=ALU.max), reads=RK, writes=RK)
                V('dve', lambda e: e.tensor_tensor(out=rC[:, :, :], in0=rA[:, :, :], in1=bc(sc(4), 16), op=ALU.is_ge), reads=RK, writes=RK)
                V('dve', lambda e: e.tensor_tensor(out=sc(5), in0=sc(4), in1=sc(3), op=ALU.subtract), reads=RK, writes=RK)
                V('act', lambda e: e.activation(out=sc(6), in_=sc(5), func=AF.Exp), reads=RK, writes=RK)
                V('dve', lambda e: e.tensor_scalar(out=sc(7), in0=sc(6), scalar1=1.0, scalar2=None, op0=ALU.add), reads=RK, writes=RK)
                V('dve', lambda e: e.reciprocal(out=sc(7), in_=sc(7)), reads=RK, writes=RK)
                V('dve', lambda e: e.tensor_tensor(out=sc(8), in0=sc(7), in1=sc(2), op=ALU.mult), reads=RK, writes=RK)
                V('dve', lambda e: e.tensor_tensor(out=sc(9), in0=sc(8), in1=sc(6), op=ALU.mult), reads=RK, writes=RK)
                V('dve', lambda e: e.tensor_tensor(out=rB[:, :, :], in0=rB[:, :, :], in1=bc(sc(8), 16), op=ALU.mult), reads=RK, writes=RK)
                V('dve', lambda e: e.tensor_tensor(out=rC[:, :, :], in0=rC[:, :, :], in1=bc(sc(9), 16), op=ALU.mult), reads=RK, writes=RK)
                V('dve', lambda e: e.tensor_tensor(out=comb[:, :, :], in0=rB[:, :, :], in1=rC[:, :, :], op=ALU.add),
                  reads=RK, writes=[('comb', tt) for tt in range(NT)])
                Wp, kp = wpanel(w_p_d[l], 2, D)
                pcnt2 = 0
                for hf in range(2):
                    Wpg, kpg = wpanel(w_pg_d[l, :, hf * 512:(hf + 1) * 512], KC, 512)
                    for tt in range(NT):
                        b = pcnt2 % 2
                        pw = 1 + pcnt2 % 2
                        pg3 = 3 + pcnt2 % 3
                        pcnt2 += 1
                        for k in range(2):
                            mm(ps[pw][:, :], pTall[:, k, tt * 128:(tt + 1) * 128], Wp[:, k, hf * 512:(hf + 1) * 512], k == 0, k == 1,
                               [kp, ('pT', tt)], [('ps', pw)])
                        for k in range(KC):
                            mm(ps[pg3][:, :], xT[:, k, tt * 128:(tt + 1) * 128], Wpg[:, k, :], k == 0, False,
                               [kpg, ('xT', tt)], [('ps', pg3)])
                        mm(ps[pg3][:, :], ones_b[0:1, :], bpg_h[0:1, hf * 512:(hf + 1) * 512], False, False,
                           ['ones_b', 'bpg_h'], [('ps', pg3)])
                        mm(ps[pg3][:, :], ones_b[0:1, :], bpg_l[0:1, hf * 512:(hf + 1) * 512], False, True,
                           ['ones_b', 'bpg_l'], [('ps', pg3)])
                        Sx.op('act', lambda e: e.activation(out=sgp[b][:, :], in_=ps[pg3][:, :], func=AF.Sigmoid),
                              reads=[('ps', pg3)], writes=[('sgp', b)])
                        Sx.op('dve', lambda e: e.tensor_tensor(out=tpb[b][:, :], in0=ps[pw][:, :], in1=sgp[b][:, :], op=ALU.mult),
                              reads=[('ps', pw), ('sgp', b)], writes=[('tpb', b)])
                        Sx.op('dve', lambda e: e.scalar_tensor_tensor(
                            out=acc[:, tt, hf * 512:(hf + 1) * 512], in0=acc[:, tt, hf * 512:(hf + 1) * 512], scalar=DN_ALPHA,
                            in1=tpb[b][:, :], op0=ALU.mult, op1=ALU.add),
                            reads=[('tpb', b), ('acc', tt, hf)], writes=[('acc', tt, hf)])
                mstate = {'hcnt': 0, 'ycnt': 0, 'W': None}

                def moe_h(u):
                    ex, tb = u // NB, u % NB
                    if tb == 0:
                        W1, k1 = wpanel(w1_d[l, ex], KC, 512)
                        W3, k3 = wpanel(w3_d[l, ex], KC, 512)
                        W2, k2 = wpanel(w2_d[l, ex], 4, D)
                        mstate[('W', ex)] = (W1, k1, W3, k3, W2, k2)
                    W1, k1, W3, k3, W2, k2 = mstate[('W', ex)]
                    hb = u % 2
                    for fc in range(4):
                        hcnt = mstate['hcnt']
                        p1, p3 = (hcnt % 2), 2 + (hcnt % 2)
                        s1 = s1b[hcnt % 2]
                        s1k = ('s1', hcnt % 2)
                        mstate['hcnt'] += 1
                        for k in range(KC):
                            mm(ps[p1][:, :], W1[:, k, fc * 128:(fc + 1) * 128], xT[:, k, tb * 512:(tb + 1) * 512],
                               k == 0, k == KC - 1, [k1] + xT_keys(tb), [('ps', p1)])
                        for k in range(KC):
                            mm(ps[p3][:, :], W3[:, k, fc * 128:(fc + 1) * 128], xT[:, k, tb * 512:(tb + 1) * 512],
                               k == 0, k == KC - 1, [k3] + xT_keys(tb), [('ps', p3)])
                        Sx.op('act', lambda e: e.activation(out=s1[:, :], in_=ps[p1][:, :], func=AF.Silu),
                              reads=[('ps', p1)], writes=[s1k])
                        Sx.op('dve', lambda e: e.tensor_tensor(
                            out=hT[hb][:, fc, :], in0=ps[p3][:, :], in1=s1[:, :], op=ALU.mult),
                            reads=[('ps', p3), s1k], writes=[('hT', hb, fc)])

                def moe_y(u):
                    ex, tb = u // NB, u % NB
                    W1, k1, W3, k3, W2, k2 = mstate[('W', ex)]
                    hb = u % 2
                    for ts in range(4):
                        tt = tb * 4 + ts
                        for hf in range(2):
                            py = 4 + (mstate['ycnt'] % 4)
                            mstate['ycnt'] += 1
                            for fc in range(4):
                                mm(ps[py][:, :], hT[hb][:, fc, ts * 128:(ts + 1) * 128], W2[:, fc, hf * 512:(hf + 1) * 512],
                                   fc == 0, fc == 3, [k2, ('hT', hb, fc)], [('ps', py)])
                            Sx.op('dve', lambda e: e.scalar_tensor_tensor(
                                out=acc[:, tt, hf * 512:(hf + 1) * 512], in0=ps[py][:, :], scalar=comb[:, tt, ex:ex + 1],
                                in1=acc[:, tt, hf * 512:(hf + 1) * 512], op0=ALU.mult, op1=ALU.add),
                                reads=[('ps', py), ('comb', tt), ('acc', tt, hf)], writes=[('acc', tt, hf)])

                NU = 16 * NB
                for u in range(NU + 1):
                    if u < NU:
                        moe_h(u)
                    if u >= 1:
                        moe_y(u - 1)
                Sx.barrier()
                wreset()
                ln = ln_bufs()
                load_gb(bco[('ln2_g', l)], bco[('ln2_b', l)])
                final = last_layer
                ln_phase(ln, lambda tt: (acc[:, tt, :], [('acc', tt, 0), ('acc', tt, 1)]), None,
                         lambda tt: ((out_d if final else xs_d)[s, tt * 128:(tt + 1) * 128, :],
                                     ('out', s, tt) if final else ('xs', tt)),
                         not final, final, NT)
        Sx.finish('sp')
        Sx.emit()
    return nc


def prep_shared(inp, L=DEPTH):
    perm = q_perm()
    w_in = np.array(inp['w_in'], dtype=np.float32, copy=True)
    b_in = np.array(inp['b_in'], dtype=np.float32, copy=True)
    w_in[:, :, 2048:3072] = w_in[:, :, 2048:3072][:, :, perm]
    b_in[:, 2048:3072] = b_in[:, 2048:3072][:, perm]
    w_ao = np.ascontiguousarray(np.asarray(inp['w_attn_out'], dtype=np.float32)[:, perm, :])
    w_r = np.zeros((L, D, 32), dtype=np.float32)
    w_r[:, :, 0:4] = inp['w_router_group']
    w_r[:, :, 4:20] = inp['w_router_expert']
    fmo, NV = fm_layout(L)
    bco, NBC = bc_layout(L)
    vfm = np.zeros((128, NV), dtype=np.float32)
    vbc = np.zeros((NBC,), dtype=np.float32)

    def fm(v):
        return np.asarray(v, dtype=np.float32).reshape(-1, 128).T

    for l in range(L):
        vfm[:, fmo[('b_in', l)]:fmo[('b_in', l)] + 44] = fm(b_in[l])
        cw = np.asarray(inp['conv_w'][l], dtype=np.float32).T.reshape(8, 128, 31).transpose(1, 0, 2).reshape(128, 248)
        vfm[:, fmo[('conv_w', l)]:fmo[('conv_w', l)] + 248] = cw
        vfm[:, fmo[('conv_b', l)]:fmo[('conv_b', l)] + 8] = fm(inp['conv_b'][l])
        vfm[:, fmo[('cln_g', l)]:fmo[('cln_g', l)] + 8] = fm(inp['conv_ln_g'][l])
        vfm[:, fmo[('cln_b', l)]:fmo[('cln_b', l)] + 8] = fm(inp['conv_ln_b'][l])
        for nm, src in (('ln1_g', 'ln1_g'), ('ln1_b', 'ln1_b'), ('ln2_g', 'ln2_g'), ('ln2_b', 'ln2_b'), ('b_pg', 'b_pg')):
            vbc[bco[(nm, l)]:bco[(nm, l)] + D] = inp[src][l]
        vbc[bco[('b_v', l)]:bco[('b_v', l)] + 256] = b_in[l, 3328:3584]
        vbc[bco[('b_r', l)]:bco[('b_r', l)] + 4] = inp['b_router_group'][l]
        vbc[bco[('b_r', l)] + 4:bco[('b_r', l)] + 20] = inp['b_router_expert'][l]
        vbc[bco[('sink', l)]:bco[('sink', l)] + 16] = inp['attn_sink'][l]
    vbc[bco['emb_g']:bco['emb_g'] + D] = inp['ln_emb_g']
    vbc[bco['emb_b']:bco['emb_b'] + D] = inp['ln_emb_b']
    kk = np.arange(128)[:, None].astype(np.float64)
    qq = np.arange(128)[None, :].astype(np.float64)
    Dm = np.zeros((3, 128, 128))
    Dm[0] = np.where(kk >= qq, qq - kk + 128, np.inf)
    Dm[1] = np.abs(kk - qq)
    Dm[2] = np.where(kk <= qq, kk - qq + 128, np.inf)
    import ml_dtypes
    alibi = np.zeros((2, 2, 128, 6, 4, 128), dtype=np.float32)
    for p in range(2):
        for e in range(2):
            for g in range(4):
                sl = slope(2 * p + e, g)
                for j in range(3):
                    a = -sl * Dm[j]
                    a[~np.isfinite(a)] = -1.0e4
                    a = a.astype(np.float32)
                    hi = a.astype(ml_dtypes.bfloat16).astype(np.float32)
                    lo = (a - hi).astype(ml_dtypes.bfloat16).astype(np.float32)
                    alibi[p, 0, :, e * 3 + j, g, :] = hi
                    alibi[p, 1, :, e * 3 + j, g, :] = lo
    shared = dict(
        w_in=w_in, w_conv_out=np.asarray(inp['w_conv_out'], dtype=np.float32), w_attn_out=w_ao,
        w_out=np.asarray(inp['w_out'], dtype=np.float32), w_r=w_r,
        w1=np.asarray(inp['w1'], dtype=np.float32).reshape(L, 16, D, 512),
        w3=np.asarray(inp['w3'], dtype=np.float32).reshape(L, 16, D, 512),
        w2=np.asarray(inp['w2'], dtype=np.float32).reshape(L, 16, 512, D),
        w_p=np.asarray(inp['w_p'], dtype=np.float32), w_pg=np.asarray(inp['w_pg'], dtype=np.float32),
        vfm=vfm, vbc=vbc, alibi=alibi.reshape(2, 2, 128, 6, 512).astype(ml_dtypes.bfloat16), ident=np.eye(128, dtype=np.float32))
    return shared


_NC_CACHE = {}


def run(inp, ncores, nseq, S, depth=DEPTH, trace=False, debug=False):
    key = (nseq, S, depth, debug)
    if key not in _NC_CACHE:
        _NC_CACHE[key] = build_nc(nseq, S, depth, debug)
    nc = _NC_CACHE[key]
    shared = prep_shared(inp, depth)
    x = np.asarray(inp['x'], dtype=np.float32)
    p = np.asarray(inp['p'], dtype=np.float32)
    in_maps = []
    for c in range(ncores):
        m = dict(shared)
        m['x'] = np.ascontiguousarray(x[c * nseq:(c + 1) * nseq])
        m['p'] = np.ascontiguousarray(p[:, c * nseq:(c + 1) * nseq])
        in_maps.append(m)
    res = run_bass_kernel_spmd(nc, in_maps, core_ids=list(range(ncores)), trace=trace)
    out = np.concatenate([r['out'] for r in res.results], axis=0)
    if debug:
        res.dbg = np.concatenate([r['dbg'] for r in res.results], axis=1)
    return out, res


def kernel(**inputs):
    out, _ = run(inputs, NCORES, 2, 2048, DEPTH)
    return out.astype(np.float32)
```
